# Optimizing a Trainium2 kernel written in Bass

```python
import math
import jax
import jax.numpy as jnp
from jax import lax
import numpy as np

D_MODEL = 1024
BATCH = 4
SEQ = 4096
DEPTH = 2
DEC_BATCH = 128
DEC_SEQ = 8
PAST_LEN = 2048
PAGE_SIZE = 128

BRANCH_WIDTH = D_MODEL // 2
N_BRANCH = 3
S5_WIDTH = BRANCH_WIDTH
S5_GROUP = 16
S5_GROUPS = S5_WIDTH // S5_GROUP
S5_STATE = 64
FOX_HEAD_DIM = 64
FOX_HEADS = BRANCH_WIDTH // FOX_HEAD_DIM
FOX_WIDTH = FOX_HEADS * FOX_HEAD_DIM
Q_BLOCK = 128
SSD_WIDTH = BRANCH_WIDTH
SSD_HEAD_DIM = 64
SSD_HEADS = SSD_WIDTH // SSD_HEAD_DIM
SSD_GROUPS = 2
SSD_STATE = 128
SSD_CONV = 4
SSD_CHUNK = 128
SSD_CONV_DIM = SSD_WIDTH + 2 * SSD_GROUPS * SSD_STATE
D_IN = N_BRANCH * D_MODEL + S5_WIDTH + 3 * FOX_WIDTH + FOX_HEADS + SSD_WIDTH + SSD_CONV_DIM + SSD_HEADS
D_FF = ((8 * D_MODEL // 3 + 127) // 128) * 128
N_EXPERTS = 8
TOP_K = 2
D_FF_EXPERT = D_MODEL
N_DENSE = (DEPTH + 1) // 2
N_MOE = DEPTH // 2
ALPHA = (2 * DEPTH) ** 0.25
BETA = (8 * DEPTH) ** -0.25
LN_EPS = 1e-5
RMS_EPS = 1e-5

kernel_name = 'hybrid_s5_fox_ssd_decoder_step'


def _in_split_points():
    sizes = (N_BRANCH * D_MODEL, S5_WIDTH, FOX_WIDTH, FOX_WIDTH, FOX_WIDTH, FOX_HEADS,
             SSD_WIDTH, SSD_CONV_DIM, SSD_HEADS)
    return np.cumsum(sizes)[:-1].tolist()


def layer_norm(x, g, b):
    xf = x.astype(jnp.float32)
    mu = jnp.mean(xf, axis=-1, keepdims=True)
    var = jnp.mean(jnp.square(xf - mu), axis=-1, keepdims=True)
    return ((xf - mu) * lax.rsqrt(var + LN_EPS) * g.astype(jnp.float32) + b.astype(jnp.float32)).astype(x.dtype)


def swiglu(h, w_gate, w_up, w_down):
    return (jax.nn.silu(h @ w_gate) * (h @ w_up)) @ w_down


def moe_swiglu(h, w_router, b_router, w_gate, w_up, w_down):
    logits = (h @ w_router).astype(jnp.float32) + b_router.astype(jnp.float32)
    top_val, top_idx = lax.top_k(logits, TOP_K)
    top_w = jax.nn.softmax(top_val, axis=-1)
    gate = jnp.einsum('blk,blke->ble', top_w, jax.nn.one_hot(top_idx, N_EXPERTS, dtype=jnp.float32))
    out = jnp.zeros(h.shape, jnp.float32)
    for e in range(N_EXPERTS):
        out = out + gate[..., e:e + 1] * swiglu(h, w_gate[e], w_up[e], w_down[e]).astype(jnp.float32)
    return out.astype(h.dtype)


def s5_discretise(lam_re, lam_im, log_dt, b_re, b_im):
    dt = jnp.exp(log_dt.astype(jnp.float32))[:, None]
    lr = lam_re.astype(jnp.float32)
    li = lam_im.astype(jnp.float32)
    mag = jnp.exp(lr * dt)
    ab_re = mag * jnp.cos(li * dt)
    ab_im = mag * jnp.sin(li * dt)
    nr = ab_re - 1.0
    den = lr * lr + li * li
    q_re = (nr * lr + ab_im * li) / den
    q_im = (ab_im * lr - nr * li) / den
    bb_re = q_re[..., None] * b_re - q_im[..., None] * b_im
    bb_im = q_re[..., None] * b_im + q_im[..., None] * b_re
    return ab_re, ab_im, bb_re, bb_im


def s5_branch(u, h0_re, h0_im, lam_re, lam_im, log_dt, b_re, b_im, c_re, c_im, d_skip, w_glu, b_glu):
    bsz, seq, _ = u.shape
    ug = u.astype(jnp.float32).reshape(bsz, seq, S5_GROUPS, S5_GROUP)
    ab_re, ab_im, bb_re, bb_im = s5_discretise(lam_re, lam_im, log_dt, b_re, b_im)
    bu_re = jnp.einsum('gnc,blgc->blgn', bb_re, ug)
    bu_im = jnp.einsum('gnc,blgc->blgn', bb_im, ug)
    a_re = jnp.broadcast_to(ab_re, bu_re.shape)
    a_im = jnp.broadcast_to(ab_im, bu_im.shape)

    def combine(e1, e2):
        a1r, a1i, b1r, b1i = e1
        a2r, a2i, b2r, b2i = e2
        return (a2r * a1r - a2i * a1i, a2r * a1i + a2i * a1r,
                a2r * b1r - a2i * b1i + b2r, a2r * b1i + a2i * b1r + b2i)

    pr, pi, xr, xi = lax.associative_scan(combine, (a_re, a_im, bu_re, bu_im), axis=1)
    h0r = h0_re.astype(jnp.float32)[:, None]
    h0i = h0_im.astype(jnp.float32)[:, None]
    xr = xr + pr * h0r - pi * h0i
    xi = xi + pr * h0i + pi * h0r
    y = jnp.einsum('gcn,blgn->blgc', c_re, xr) - jnp.einsum('gcn,blgn->blgc', c_im, xi)
    y = y + d_skip.astype(jnp.float32).reshape(S5_GROUPS, S5_GROUP) * ug
    y = jax.nn.gelu(y.reshape(bsz, seq, S5_WIDTH))
    y = y * jax.nn.sigmoid(y @ w_glu + b_glu)
    return y.astype(u.dtype), xr[:, -1], xi[:, -1]


def fox_prompt(q, k, v, logf):
    bsz, seq, nh, dh = q.shape
    nb = seq // Q_BLOCK
    scale = dh ** -0.5
    c = jnp.cumsum(logf, axis=1)
    c_keys = c.transpose(0, 2, 1)
    qb = q.reshape(bsz, nb, Q_BLOCK, nh, dh).transpose(1, 0, 2, 3, 4)
    cb = c.reshape(bsz, nb, Q_BLOCK, nh).transpose(1, 0, 3, 2)
    kpos = jnp.arange(seq)

    def block(args):
        i, qi, ci = args
        s = jnp.einsum('bqhd,bkhd->bhqk', qi, k).astype(jnp.float32) * scale
        s = s + ci[..., None] - c_keys[:, :, None, :]
        qpos = i * Q_BLOCK + jnp.arange(Q_BLOCK)
        s = jnp.where(kpos[None, :] <= qpos[:, None], s, -jnp.inf)
        p = jax.nn.softmax(s, axis=-1).astype(v.dtype)
        return jnp.einsum('bhqk,bkhd->bqhd', p, v)

    o = lax.map(block, (jnp.arange(nb), qb, cb))
    return o.transpose(1, 0, 2, 3, 4).reshape(bsz, seq, nh * dh)


def fox_sample(q, k, v, logf, k_past, v_past, logf_past):
    bsz, tq, nh, dh = q.shape
    n_past = k_past.shape[1]
    scale = dh ** -0.5
    cp = jnp.cumsum(logf_past.astype(jnp.float32), axis=1)
    decay_past = (cp[:, -1:] - cp).transpose(0, 2, 1)
    cn = jnp.cumsum(logf, axis=1).transpose(0, 2, 1)
    s_past = (jnp.einsum('bqhd,bkhd->bhqk', q, k_past).astype(jnp.float32) * scale
              + decay_past[:, :, None, :] + cn[..., None])
    s_new = (jnp.einsum('bqhd,bkhd->bhqk', q, k).astype(jnp.float32) * scale
             + cn[..., :, None] - cn[..., None, :])
    s_new = jnp.where(jnp.tril(jnp.ones((tq, tq), bool)), s_new, -jnp.inf)
    p = jax.nn.softmax(jnp.concatenate([s_past, s_new], axis=-1), axis=-1).astype(v.dtype)
    o = (jnp.einsum('bhqk,bkhd->bqhd', p[..., :n_past], v_past)
         + jnp.einsum('bhqk,bkhd->bqhd', p[..., n_past:], v))
    return o.reshape(bsz, tq, nh * dh)


def causal_conv(xbc, conv0, w, b):
    seq = xbc.shape[1]
    xp = jnp.concatenate([conv0.astype(xbc.dtype), xbc], axis=1)
    acc = xp[:, 0:seq] * w[0]
    for j in range(1, SSD_CONV):
        acc = acc + xp[:, j:j + seq] * w[j]
    return jax.nn.silu(acc + b), xp[:, seq:]


def segsum(a):
    t = a.shape[-1]
    x = jnp.broadcast_to(a[..., :, None], a.shape + (t,))
    x = jnp.where(jnp.tril(jnp.ones((t, t), bool), -1), x, 0.0)
    ss = jnp.cumsum(x, axis=-2)
    return jnp.where(jnp.tril(jnp.ones((t, t), bool), 0), ss, -jnp.inf)


def ssd_scan(x, dt, a, bm, cm, h0):
    bsz, seq, nh, hp = x.shape
    q = math.gcd(seq, SSD_CHUNK)
    nc = seq // q
    rep = nh // SSD_GROUPS
    bh = jnp.repeat(bm, rep, axis=2).reshape(bsz, nc, q, nh, SSD_STATE)
    ch = jnp.repeat(cm, rep, axis=2).reshape(bsz, nc, q, nh, SSD_STATE)
    xd = (x * dt[..., None]).reshape(bsz, nc, q, nh, hp)
    adt = (a * dt).reshape(bsz, nc, q, nh).transpose(0, 3, 1, 2)
    a_cum = jnp.cumsum(adt, axis=-1)
    lmat = jnp.exp(segsum(adt))
    y_diag = jnp.einsum('bclhn,bcshn,bhcls,bcshp->bclhp', ch, bh, lmat, xd)
    decay_states = jnp.exp(a_cum[..., -1:] - a_cum)
    states = jnp.einsum('bclhn,bhcl,bclhp->bchpn', bh, decay_states, xd)
    states = jnp.concatenate([h0[:, None], states], axis=1)
    decay_chunk = jnp.exp(segsum(jnp.pad(a_cum[..., -1], ((0, 0), (0, 0), (1, 0)))))
    new_states = jnp.einsum('bhzc,bchpn->bzhpn', decay_chunk, states)
    states, final = new_states[:, :-1], new_states[:, -1]
    y_off = jnp.einsum('bclhn,bchpn,bhcl->bclhp', ch, states, jnp.exp(a_cum))
    return (y_diag + y_off).reshape(bsz, seq, nh, hp), final


def ssd_branch(z, xbc, dt_raw, conv0, h0, conv_w, conv_b, dt_bias, a_log, d_skip, norm_w):
    bsz, seq, _ = z.shape
    xbc_c, conv_new = causal_conv(xbc, conv0, conv_w, conv_b)
    xbc_c = xbc_c.astype(jnp.float32)
    xs, bm, cm = jnp.split(xbc_c, [SSD_WIDTH, SSD_WIDTH + SSD_GROUPS * SSD_STATE], axis=-1)
    xs = xs.reshape(bsz, seq, SSD_HEADS, SSD_HEAD_DIM)
    bm = bm.reshape(bsz, seq, SSD_GROUPS, SSD_STATE)
    cm = cm.reshape(bsz, seq, SSD_GROUPS, SSD_STATE)
    dt = jax.nn.softplus(dt_raw.astype(jnp.float32) + dt_bias.astype(jnp.float32))
    a = -jnp.exp(a_log.astype(jnp.float32))
    y, h_new = ssd_scan(xs, dt, a, bm, cm, h0.astype(jnp.float32))
    y = y + d_skip.astype(jnp.float32)[:, None] * xs
    y = y.reshape(bsz, seq, SSD_WIDTH) * jax.nn.silu(z.astype(jnp.float32))
    y = y * lax.rsqrt(jnp.mean(jnp.square(y), axis=-1, keepdims=True) + RMS_EPS) * norm_w.astype(jnp.float32)
    return y.astype(z.dtype), conv_new, h_new


def gather_pages(cache_l, page_table):
    rows = cache_l[page_table]
    return rows.reshape((page_table.shape[0], -1) + cache_l.shape[2:])


def token_mixer(h, l, p, s5_re0, s5_im0, conv0, ssd0, past):
    bsz, seq, _ = h.shape
    gates, u, q, k, v, fg, z, xbc, dt_raw = jnp.split(h @ p['w_in'][l], _in_split_points(), axis=-1)
    gates = jax.nn.sigmoid(gates.reshape(bsz, seq, N_BRANCH, D_MODEL) + p['b_gate'][l])
    y_s5, s5_re, s5_im = s5_branch(u, s5_re0, s5_im0, p['s5_lam_re'][l], p['s5_lam_im'][l],
                                   p['s5_log_dt'][l], p['s5_b_re'][l], p['s5_b_im'][l],
                                   p['s5_c_re'][l], p['s5_c_im'][l], p['s5_d'][l],
                                   p['s5_w_glu'][l], p['s5_b_glu'][l])
    q = q.reshape(bsz, seq, FOX_HEADS, FOX_HEAD_DIM)
    k = k.reshape(bsz, seq, FOX_HEADS, FOX_HEAD_DIM)
    v = v.reshape(bsz, seq, FOX_HEADS, FOX_HEAD_DIM)
    logf = jax.nn.log_sigmoid(fg.astype(jnp.float32) + p['b_fgate'][l].astype(jnp.float32))
    if past is None:
        y_fox = fox_prompt(q, k, v, logf)
    else:
        y_fox = fox_sample(q, k, v, logf, past[0], past[1], past[2])
    y_ssd, conv_new, ssd_new = ssd_branch(z, xbc, dt_raw, conv0, ssd0, p['ssd_conv_w'][l],
                                          p['ssd_conv_b'][l], p['ssd_dt_bias'][l], p['ssd_a_log'][l],
                                          p['ssd_d'][l], p['ssd_norm_w'][l])
    merged = (gates[:, :, 0] * (y_s5 @ p['w_branch_s5'][l])
              + gates[:, :, 1] * (y_fox @ p['w_branch_fox'][l])
              + gates[:, :, 2] * (y_ssd @ p['w_branch_ssd'][l]))
    out = merged @ p['w_o'][l]
    return out, (k, v, logf, s5_re, s5_im, conv_new, ssd_new)


def run_trunk(x, p, s5_re0, s5_im0, conv0, ssd0, paged):
    new = [[] for _ in range(7)]
    for l in range(DEPTH):
        past = None
        if paged is not None:
            past = (gather_pages(paged[0][l], paged[3]), gather_pages(paged[1][l], paged[3]),
                    gather_pages(paged[2][l], paged[3]))
        m, st = token_mixer(x, l, p, s5_re0[l], s5_im0[l], conv0[l], ssd0[l], past)
        for lst, s in zip(new, st):
            lst.append(s)
        x = layer_norm(ALPHA * x + m, p['ln1_g'][l], p['ln1_b'][l])
        if l % 2 == 0:
            f = swiglu(x, p['ffn_w_gate'][l // 2], p['ffn_w_up'][l // 2], p['ffn_w_down'][l // 2])
        else:
            f = moe_swiglu(x, p['moe_w_router'][l // 2], p['moe_b_router'][l // 2],
                           p['moe_w_gate'][l // 2], p['moe_w_up'][l // 2], p['moe_w_down'][l // 2])
        x = layer_norm(ALPHA * x + f, p['ln2_g'][l], p['ln2_b'][l])
    return x, [jnp.stack(s) for s in new]


def setup_inputs(seed: int = 0) -> dict:
    key = jax.random.key(seed)
    keys = jax.random.split(key, 64)
    counter = [0]

    def nk():
        counter[0] += 1
        return keys[counter[0] - 1]

    def nrm(shape, scale):
        return jax.random.normal(nk(), shape, jnp.float32) * scale

    def unif(shape, lo, hi):
        return jax.random.uniform(nk(), shape, jnp.float32, lo, hi)

    n_pages = PAST_LEN // PAGE_SIZE
    n_used = DEC_BATCH * n_pages
    n_pool = n_used + max(1, n_used // 4)
    page_table = jax.random.permutation(nk(), n_pool)[:n_used].reshape(DEC_BATCH, n_pages).astype(jnp.int32)

    x_prompt = nrm((BATCH, SEQ, D_MODEL), 1.0)
    x_sample = nrm((DEC_BATCH, DEC_SEQ, D_MODEL), 1.0)
    cache_k = nrm((DEPTH, n_pool, PAGE_SIZE, FOX_HEADS, FOX_HEAD_DIM), 1.0)
    cache_v = nrm((DEPTH, n_pool, PAGE_SIZE, FOX_HEADS, FOX_HEAD_DIM), 1.0)
    cache_logf = jax.nn.log_sigmoid(nrm((DEPTH, n_pool, PAGE_SIZE, FOX_HEADS), 1.0) + 2.5)
    state_s5_re = nrm((DEPTH, DEC_BATCH, S5_GROUPS, S5_STATE), 0.5)
    state_s5_im = nrm((DEPTH, DEC_BATCH, S5_GROUPS, S5_STATE), 0.5)
    state_conv = nrm((DEPTH, DEC_BATCH, SSD_CONV - 1, SSD_CONV_DIM), 1.0)
    state_ssd = nrm((DEPTH, DEC_BATCH, SSD_HEADS, SSD_HEAD_DIM, SSD_STATE), 0.1)

    w_in = nrm((DEPTH, D_MODEL, D_IN), D_MODEL ** -0.5)
    b_gate = nrm((DEPTH, N_BRANCH, D_MODEL), 0.01)
    b_fgate = unif((DEPTH, FOX_HEADS), 1.0, 4.0)

    s5_lam_re = -0.5 + nrm((DEPTH, S5_GROUPS, S5_STATE), 0.01)
    s5_lam_im = jnp.pi * jnp.arange(S5_STATE, dtype=jnp.float32) + nrm((DEPTH, S5_GROUPS, S5_STATE), 0.01)
    s5_log_dt = unif((DEPTH, S5_GROUPS), math.log(1e-3), math.log(1e-1))
    s5_b_re = nrm((DEPTH, S5_GROUPS, S5_STATE, S5_GROUP), (2 * S5_GROUP) ** -0.5)
    s5_b_im = nrm((DEPTH, S5_GROUPS, S5_STATE, S5_GROUP), (2 * S5_GROUP) ** -0.5)
    s5_c_re = nrm((DEPTH, S5_GROUPS, S5_GROUP, S5_STATE), (2 * S5_STATE) ** -0.5)
    s5_c_im = nrm((DEPTH, S5_GROUPS, S5_GROUP, S5_STATE), (2 * S5_STATE) ** -0.5)
    s5_d = nrm((DEPTH, S5_WIDTH), 1.0)
    s5_w_glu = nrm((DEPTH, S5_WIDTH, S5_WIDTH), S5_WIDTH ** -0.5)
    s5_b_glu = nrm((DEPTH, S5_WIDTH), 0.01)

    ssd_conv_w = nrm((DEPTH, SSD_CONV, SSD_CONV_DIM), SSD_CONV ** -0.5)
    ssd_conv_b = nrm((DEPTH, SSD_CONV_DIM), 0.01)
    dt0 = jnp.exp(unif((DEPTH, SSD_HEADS), math.log(1e-3), math.log(1e-1)))
    ssd_dt_bias = dt0 + jnp.log(-jnp.expm1(-dt0))
    ssd_a_log = jnp.log(unif((DEPTH, SSD_HEADS), 1.0, 16.0))
    ssd_d = 1.0 + nrm((DEPTH, SSD_HEADS), 0.1)
    ssd_norm_w = 1.0 + nrm((DEPTH, SSD_WIDTH), 0.1)

    w_branch_s5 = nrm((DEPTH, S5_WIDTH, D_MODEL), S5_WIDTH ** -0.5)
    w_branch_fox = nrm((DEPTH, FOX_WIDTH, D_MODEL), FOX_WIDTH ** -0.5)
    w_branch_ssd = nrm((DEPTH, SSD_WIDTH, D_MODEL), SSD_WIDTH ** -0.5)
    w_o = nrm((DEPTH, D_MODEL, D_MODEL), BETA * D_MODEL ** -0.5)

    ln1_g = 1.0 + nrm((DEPTH, D_MODEL), 0.1)
    ln1_b = nrm((DEPTH, D_MODEL), 0.01)
    ln2_g = 1.0 + nrm((DEPTH, D_MODEL), 0.1)
    ln2_b = nrm((DEPTH, D_MODEL), 0.01)

    ffn_w_gate = nrm((N_DENSE, D_MODEL, D_FF), D_MODEL ** -0.5)
    ffn_w_up = nrm((N_DENSE, D_MODEL, D_FF), D_MODEL ** -0.5)
    ffn_w_down = nrm((N_DENSE, D_FF, D_MODEL), BETA * D_FF ** -0.5)
    moe_w_router = nrm((N_MOE, D_MODEL, N_EXPERTS), D_MODEL ** -0.5)
    moe_b_router = nrm((N_MOE, N_EXPERTS), 0.01)
    moe_w_gate = nrm((N_MOE, N_EXPERTS, D_MODEL, D_FF_EXPERT), D_MODEL ** -0.5)
    moe_w_up = nrm((N_MOE, N_EXPERTS, D_MODEL, D_FF_EXPERT), D_MODEL ** -0.5)
    moe_w_down = nrm((N_MOE, N_EXPERTS, D_FF_EXPERT, D_MODEL), BETA * D_FF_EXPERT ** -0.5)

    return {'x_prompt': x_prompt, 'x_sample': x_sample, 'cache_k': cache_k, 'cache_v': cache_v,
            'cache_logf': cache_logf, 'page_table': page_table, 'state_s5_re': state_s5_re,
            'state_s5_im': state_s5_im, 'state_conv': state_conv, 'state_ssd': state_ssd,
            'w_in': w_in, 'b_gate': b_gate, 'b_fgate': b_fgate,
            's5_lam_re': s5_lam_re, 's5_lam_im': s5_lam_im, 's5_log_dt': s5_log_dt,
            's5_b_re': s5_b_re, 's5_b_im': s5_b_im, 's5_c_re': s5_c_re, 's5_c_im': s5_c_im,
            's5_d': s5_d, 's5_w_glu': s5_w_glu, 's5_b_glu': s5_b_glu,
            'ssd_conv_w': ssd_conv_w, 'ssd_conv_b': ssd_conv_b, 'ssd_dt_bias': ssd_dt_bias,
            'ssd_a_log': ssd_a_log, 'ssd_d': ssd_d, 'ssd_norm_w': ssd_norm_w,
            'w_branch_s5': w_branch_s5, 'w_branch_fox': w_branch_fox, 'w_branch_ssd': w_branch_ssd,
            'w_o': w_o, 'ln1_g': ln1_g, 'ln1_b': ln1_b, 'ln2_g': ln2_g, 'ln2_b': ln2_b,
            'ffn_w_gate': ffn_w_gate, 'ffn_w_up': ffn_w_up, 'ffn_w_down': ffn_w_down,
            'moe_w_router': moe_w_router, 'moe_b_router': moe_b_router, 'moe_w_gate': moe_w_gate,
            'moe_w_up': moe_w_up, 'moe_w_down': moe_w_down}


def reference(x_prompt, x_sample, cache_k, cache_v, cache_logf, page_table, state_s5_re, state_s5_im,
              state_conv, state_ssd, w_in, b_gate, b_fgate, s5_lam_re, s5_lam_im, s5_log_dt, s5_b_re,
              s5_b_im, s5_c_re, s5_c_im, s5_d, s5_w_glu, s5_b_glu, ssd_conv_w, ssd_conv_b, ssd_dt_bias,
              ssd_a_log, ssd_d, ssd_norm_w, w_branch_s5, w_branch_fox, w_branch_ssd, w_o, ln1_g, ln1_b,
              ln2_g, ln2_b, ffn_w_gate, ffn_w_up, ffn_w_down, moe_w_router, moe_b_router, moe_w_gate,
              moe_w_up, moe_w_down):
    p = {'w_in': w_in, 'b_gate': b_gate, 'b_fgate': b_fgate, 's5_lam_re': s5_lam_re,
         's5_lam_im': s5_lam_im, 's5_log_dt': s5_log_dt, 's5_b_re': s5_b_re, 's5_b_im': s5_b_im,
         's5_c_re': s5_c_re, 's5_c_im': s5_c_im, 's5_d': s5_d, 's5_w_glu': s5_w_glu,
         's5_b_glu': s5_b_glu, 'ssd_conv_w': ssd_conv_w, 'ssd_conv_b': ssd_conv_b,
         'ssd_dt_bias': ssd_dt_bias, 'ssd_a_log': ssd_a_log, 'ssd_d': ssd_d, 'ssd_norm_w': ssd_norm_w,
         'w_branch_s5': w_branch_s5, 'w_branch_fox': w_branch_fox, 'w_branch_ssd': w_branch_ssd,
         'w_o': w_o, 'ln1_g': ln1_g, 'ln1_b': ln1_b, 'ln2_g': ln2_g, 'ln2_b': ln2_b,
         'ffn_w_gate': ffn_w_gate, 'ffn_w_up': ffn_w_up, 'ffn_w_down': ffn_w_down,
         'moe_w_router': moe_w_router, 'moe_b_router': moe_b_router, 'moe_w_gate': moe_w_gate,
         'moe_w_up': moe_w_up, 'moe_w_down': moe_w_down}
    bsz = x_prompt.shape[0]
    z_s5 = jnp.zeros((DEPTH, bsz, S5_GROUPS, S5_STATE), jnp.float32)
    z_conv = jnp.zeros((DEPTH, bsz, SSD_CONV - 1, SSD_CONV_DIM), x_prompt.dtype)
    z_ssd = jnp.zeros((DEPTH, bsz, SSD_HEADS, SSD_HEAD_DIM, SSD_STATE), jnp.float32)
    y_prompt, st_p = run_trunk(x_prompt, p, z_s5, z_s5, z_conv, z_ssd, None)
    y_sample, st_s = run_trunk(x_sample, p, state_s5_re, state_s5_im, state_conv, state_ssd,
                               (cache_k, cache_v, cache_logf, page_table))
    k_p, v_p, lf_p, s5r_p, s5i_p, conv_p, ssd_p = st_p
    k_s, v_s, lf_s, s5r_s, s5i_s, conv_s, ssd_s = st_s
    return (y_prompt, y_sample, k_p, v_p, lf_p, s5r_p, s5i_p, conv_p, ssd_p,
            k_s, v_s, lf_s, s5r_s, s5i_s, conv_s, ssd_s)
```

```python
import math
from contextlib import ExitStack

import numpy as np
import concourse.bass as bass
import concourse.mybir as mybir
from concourse.bass_utils import run_bass_kernel_spmd

F32 = mybir.dt.float32
BF16 = mybir.dt.bfloat16
I32 = mybir.dt.int32
AF = mybir.ActivationFunctionType
ALU = mybir.AluOpType
PI = math.pi

D = 1024
DIN = 6672
DFF = 2816
NE = 8
DEPTH = 2
ALPHA = (2 * DEPTH) ** 0.25
LN_EPS = 1e-5
RMS_EPS = 1e-5
C_GATE, C_U, C_Q, C_K, C_V, C_F, C_Z, C_XBC, C_DT = 0, 3072, 3584, 4096, 4608, 5120, 5128, 5640, 6664
NEG = -30000.0


class Cfg:
    def __init__(self, SEQ=4096, TT=512, NPG=16, NPOOL=2560, stop=None, skip_prompt=False, variant=0):
        self.SEQ, self.TT, self.NPG, self.NPOOL, self.stop = SEQ, TT, NPG, NPOOL, stop
        self.skip_prompt, self.variant = skip_prompt, variant


class _Stop(Exception):
    pass


class Buf:
    def __init__(self, t, name, pend=None, base=None):
        self.t = t
        self.name = name
        self.base = base or name
        self.lw = None
        self.psum = False
        self.rd = dict(pend) if pend else {}

    def __getitem__(self, k):
        return self.t[k]


class KB:
    def __init__(self, nc, es):
        self.nc, self.es = nc, es
        self.eng = {'pe': nc.tensor, 'act': nc.scalar, 'dve': nc.vector, 'pool': nc.gpsimd, 'sp': nc.sync}
        self.sem = {e: es.enter_context(nc.semaphore("sem_" + e)) for e in self.eng}
        self.cnt = {e: 0 for e in self.eng}
        self.known = {e: {} for e in self.eng}
        self.reg = {}
        self.pending = {}
        self.uid = 0
        self.nsem = 5
        self.dsems = {}
        self.live = set()
        self.open = []

    def _mk(self, es, kind, name, shape, dt):
        self.uid += 1
        nm = "%s_%d" % (name, self.uid)
        if kind == 'sb':
            t = es.enter_context(self.nc.sbuf_tensor(nm, list(shape), dt))
        else:
            t = es.enter_context(self.nc.psum_tensor(nm, list(shape), dt))
        k = 0
        while (name, k) in self.live:
            k += 1
        self.live.add((name, k))
        b = Buf(t, nm, self.pending, base="%s.%d" % (name, k))
        b.livekey = (name, k)
        b.psum = (kind == 'ps')
        self.reg[nm] = b
        return b

    def sb(self, name, shape, dt):
        return self._mk(self.es, 'sb', name, shape, dt)

    def ps(self, name, shape, dt):
        return self._mk(self.es, 'ps', name, shape, dt)

    def dram(self, name, shape, dt, kind="Internal"):
        t = self.nc.dram_tensor(name, list(shape), dt, kind=kind)
        b = Buf(t.ap(), name)
        self.reg[name] = b
        return b

    def scope(self):
        return Scope(self)

    def _buf(self, ap):
        try:
            return self.reg.get(ap.tensor.name)
        except AttributeError:
            return None

    def _wait(self, e, key, sem, val):
        if self.known[e].get(key, 0) >= val:
            return
        self.eng[e].wait_ge(sem, val)
        self.known[e][key] = val

    def _deps(self, e, rb, wb):
        for b in rb:
            if b.lw is not None:
                k, s, v = b.lw
                if not (e == 'pe' and k == 'pe'):
                    self._wait(e, k, s, v)
            if b.psum:
                for k, (s, v) in b.rd.items():
                    if k != e:
                        self._wait(e, k, s, v)
        for b in wb:
            if b.lw is not None:
                k, s, v = b.lw
                if not (e == 'pe' and k == 'pe'):
                    self._wait(e, k, s, v)
            for k, (s, v) in b.rd.items():
                if not (e == 'pe' and k == 'pe'):
                    self._wait(e, k, s, v)

    def _bufs(self, aps):
        out = []
        for a in aps:
            if a is None or isinstance(a, (int, float)):
                continue
            b = a if isinstance(a, Buf) else self._buf(a)
            if b is not None and b not in out:
                out.append(b)
        return out

    def op(self, e, fn, outs, ins):
        wb = self._bufs(outs)
        rb = [b for b in self._bufs(ins) if b not in wb]
        self._deps(e, rb, wb)
        inst = fn(self.eng[e])
        self.cnt[e] += 1
        inst.then_inc(self.sem[e], 1)
        tok = (self.sem[e], self.cnt[e])
        for b in wb:
            b.lw = (e, tok[0], tok[1])
            b.rd = {}
        for b in rb:
            b.rd[e] = tok
        return inst

    def _dma_done(self, q, inst, dest, rb):
        dk = (dest.base, q)
        if dk not in self.dsems:
            self.dsems[dk] = [self.es.enter_context(self.nc.semaphore("d%s_%s" % (q, dest.base))), 0]
            self.nsem += 1
        ent = self.dsems[dk]
        ent[1] += 16
        inst.then_inc(ent[0], 16)
        key = ('dma', dest.base, q)
        dest.lw = (key, ent[0], ent[1])
        dest.rd = {}
        for b in rb:
            b.rd[key] = (ent[0], ent[1])
        return inst

    def dma(self, q, out, in_, extra_r=(), **kw):
        wb = self._bufs([out])
        rb = [b for b in self._bufs([in_] + list(extra_r)) if b not in wb]
        self._deps(q, rb, wb)
        inst = self.eng[q].dma_start(out=out, in_=in_, **kw)
        return self._dma_done(q, inst, wb[0], rb)

    def gather(self, out, in_, idx_ap):
        q = 'pool'
        wb = self._bufs([out])
        rb = [b for b in self._bufs([idx_ap]) if b not in wb]
        self._deps(q, rb, wb)
        inst = self.nc.gpsimd.indirect_dma_start(
            out=out, out_offset=None, in_=in_,
            in_offset=bass.IndirectOffsetOnAxis(ap=idx_ap, axis=0))
        r = self._dma_done(q, inst, wb[0], rb)
        k, s_, v = wb[0].lw
        self._wait('pool', k, s_, v)
        return r

    def drain(self):
        for e in self.eng:
            if e != 'sp' and self.cnt[e] > 0:
                self._wait('sp', e, self.sem[e], self.cnt[e])
        for (base, q), ent in self.dsems.items():
            self._wait('sp', ('dma', base, q), ent[0], ent[1])

    def barrier(self, engs=('pe', 'act', 'dve', 'pool')):
        snap = {e: self.cnt[e] for e in engs}
        for e in engs:
            for o in engs:
                if o != e and snap[o] > 0:
                    self._wait(e, o, self.sem[o], snap[o])

    def finish(self, bufs):
        for b in bufs:
            if b.lw is not None:
                k, s, v = b.lw
                self._wait('sp', k, s, v)

    def mm(self, out, lhsT, rhs, start=True, stop=True):
        return self.op('pe', lambda e: e.matmul(out, lhsT=lhsT, rhs=rhs, start=start, stop=stop), [out], [lhsT, rhs])

    def tr(self, out, in_, ident):
        return self.op('pe', lambda e: e.transpose(out=out, in_=in_, identity=ident), [out], [in_, ident])

    def act(self, out, in_, func, bias=None, scale=None, eng='act'):
        kw = {}
        if bias is not None:
            kw['bias'] = bias
        if scale is not None:
            kw['scale'] = scale
        return self.op('act', lambda e: e.activation(out=out, in_=in_, func=func, **kw), [out], [in_, bias, scale])

    def tt(self, e, out, in0, in1, op):
        return self.op(e, lambda g: g.tensor_tensor(out=out, in0=in0, in1=in1, op=op), [out], [in0, in1])

    def ts(self, e, out, in0, s1, op0, s2=None, op1=None):
        if op1 is None:
            return self.op(e, lambda g: g.tensor_scalar(out=out, in0=in0, scalar1=s1, scalar2=None, op0=op0), [out], [in0, s1])
        return self.op(e, lambda g: g.tensor_scalar(out=out, in0=in0, scalar1=s1, scalar2=s2, op0=op0, op1=op1), [out], [in0, s1, s2])

    def stt(self, e, out, in0, sc, in1, op0, op1):
        return self.op(e, lambda g: g.scalar_tensor_tensor(out=out, in0=in0, scalar=sc, in1=in1, op0=op0, op1=op1), [out], [in0, sc, in1])

    def cp(self, e, out, in_):
        if e == 'act':
            return self.op('act', lambda g: g.copy(out=out, in_=in_), [out], [in_])
        return self.op(e, lambda g: g.tensor_copy(out=out, in_=in_), [out], [in_])

    def memset(self, e, out, val):
        return self.op(e, lambda g: g.memset(out, val), [out], [])

    def scan(self, e, out, d0, d1, init):
        return self.op(e, lambda g: g.tensor_tensor_scan(out=out, data0=d0, data1=d1, initial=init, op0=ALU.mult, op1=ALU.add), [out], [d0, d1, init])


class Scope:
    def __init__(self, kb):
        self.kb = kb
        self.bufs = []

    def __enter__(self):
        self.es = ExitStack()
        self.es.__enter__()
        self.kb.open.append(self)
        return self

    def sb(self, name, shape, dt):
        b = self.kb._mk(self.es, 'sb', name, shape, dt)
        self.bufs.append(b)
        return b

    def __exit__(self, *a):
        p = self.kb.pending
        for b in self.bufs:
            toks = dict(b.rd)
            if b.lw is not None and (b.lw[0] not in toks or toks[b.lw[0]][1] < b.lw[2]):
                toks[b.lw[0]] = (b.lw[1], b.lw[2])
            for k, (s, v) in toks.items():
                if k not in p or p[k][1] < v:
                    p[k] = (s, v)
            self.kb.reg.pop(b.name, None)
            self.kb.live.discard(b.livekey)
        if self in self.kb.open:
            self.kb.open.remove(self)
        self.es.__exit__(None, None, None)
        return False


def make_consts(cfg):
    NPG = cfg.NPG
    p = np.arange(128)[:, None]
    f = np.arange(128)[None, :]
    A, B = [], []
    ca, cbm = {}, {}

    def add(lst, cols, name, arr):
        arr = np.asarray(arr, dtype=np.float32).reshape(128, -1)
        cols[name] = sum(x.shape[1] for x in lst)
        lst.append(arr)

    add(A, ca, 'ident', p == f)
    add(A, ca, 'tri_p', p <= f)
    add(A, ca, 'tri_s', (p <= f) & (p // 8 == f // 8))
    add(A, ca, 'bd_s', p // 8 == f // 8)
    add(A, ca, 'ones', np.ones((128, 128)))
    add(A, ca, 'iota', np.broadcast_to(f, (128, 128)))
    add(A, ca, 'inb', p // 8 == np.arange(16)[None, :])
    add(A, ca, 'notfirst', np.broadcast_to(f % 8 != 0, (128, 128)))
    e = np.arange(128)
    add(A, ca, 'subd', (e[:, None] // NPG == e[None, :] // NPG) & (e[:, None] % NPG > e[None, :] % NPG))
    add(A, ca, 'piota', p)
    add(B, cbm, 'ident', p == f)
    add(B, cbm, 'ones', np.ones((128, 128)))
    sel = np.zeros((128, 8, 128), np.float32)
    for h in range(8):
        sel[[h, 32 + h, 64 + h], h, :] = 1.0
    add(B, cbm, 'selh', sel)
    q = np.arange(512)[None, :]
    md = np.zeros((128, 4, 512), np.float32)
    for j in range(4):
        md[:, j, :] = np.where(128 * j + p <= q, 0.0, NEG)
    add(B, cbm, 'maskdiag', md)
    add(B, cbm, 'masknew', np.where((p // 8 == f // 8) & (p <= f), 0.0, NEG))
    return np.concatenate(A, 1), ca, np.concatenate(B, 1), cbm


WEIGHT_SPECS = [
    ('w_in', 2, D * DIN), ('s5_w_glu', 2, 512 * 512), ('w_branch_s5', 2, 512 * D), ('w_branch_fox', 2, 512 * D),
    ('w_branch_ssd', 2, 512 * D), ('w_o', 2, D * D), ('ffn_w_gate', 1, D * DFF), ('ffn_w_up', 1, D * DFF),
    ('ffn_w_down', 1, DFF * D), ('moe_w_router', 1, D * NE), ('moe_w_gate', 1, NE * D * D), ('moe_w_up', 1, NE * D * D),
    ('moe_w_down', 1, NE * D * D)]

SMALL_SPECS = [('b_gate', [2, 3072]), ('b_fgate', [2, 8]), ('s5_lam_re', [2, 2048]), ('s5_lam_im', [2, 2048]),
               ('s5_log_dt', [2, 32]), ('s5_b_re', [2, 2048, 16]), ('s5_b_im', [2, 2048, 16]),
               ('s5_c_re', [2, 512, 64]), ('s5_c_im', [2, 512, 64]), ('s5_d', [2, 512]), ('s5_b_glu', [2, 512]),
               ('ssd_conv_w', [2, 4, 1024]), ('ssd_conv_b', [2, 1024]), ('ssd_dt_bias', [2, 8]), ('ssd_a_log', [2, 8]),
               ('ssd_d', [2, 8]), ('ssd_norm_w', [2, 512]), ('ln1_g', [2, 1024]), ('ln1_b', [2, 1024]),
               ('ln2_g', [2, 1024]), ('ln2_b', [2, 1024]), ('moe_b_router', [1, 8])]

NW = 4
PC = {}
_o = 0
for _n, _w in [('bg', 24), ('s5d', 4), ('bglu', 4), ('convw', 32), ('convb', 8), ('nw', 4), ('dcol', 4),
               ('bfg', 8), ('dtb', 8), ('arow', 8), ('brow', 8)]:
    PC[_n] = _o
    _o += _w
NPAR = _o


def build(cfg):
    SEQ, TT, NPG, NPOOL = cfg.SEQ, cfg.TT, cfg.NPG, cfg.NPOOL
    NT, NKB = SEQ // TT, SEQ // 128
    PGT = 16 * NPG
    nPT = (PGT + 127) // 128
    cA, ca, cB, cbm = make_consts(cfg)
    NCA, NCB = cA.shape[1], cB.shape[1]
    nc = bass.Bass("TRN2", target_bir_lowering=False)

    def din(name, shape, dt=F32):
        return nc.dram_tensor(name, list(shape), dt, kind="ExternalInput").ap()

    IN = {}
    IN['xp'] = din('xp', [SEQ, D])
    IN['xsm'] = din('xsm', [128, D])
    IN['ck'] = [din('ck%d' % i, [NPOOL * 128, 512]) for i in range(2)]
    IN['cv'] = [din('cv%d' % i, [NPOOL * 128, 512]) for i in range(2)]
    IN['clf'] = [din('clf%d' % i, [NPOOL, 1024]) for i in range(2)]
    IN['ptab'] = din('ptab', [1, PGT], I32)
    IN['s5re0'] = din('s5re0', [2, 16, 2048])
    IN['s5im0'] = din('s5im0', [2, 16, 2048])
    IN['conv0'] = din('conv0', [2, 48, 1024])
    IN['ssd0'] = din('ssd0', [2, 16, 512, 128])
    for name, cnt, n in WEIGHT_SPECS:
        IN[name] = din(name, [cnt, n])
    for name, shp in SMALL_SPECS:
        IN[name] = din(name, shp)
    IN['cstA'] = din('cstA', [128, NCA])
    IN['cstB'] = din('cstB', [128, NCB])

    es = ExitStack()
    with es:
        kb = KB(nc, es)
        OUT = {}
        for name, shp in [('y_p', [SEQ, D]), ('y_s', [128, D]), ('k_p', [2, SEQ, 512]), ('v_p', [2, SEQ, 512]),
                          ('lf_p', [2, SEQ, 8]), ('s5re_p', [2, 2048]), ('s5im_p', [2, 2048]), ('conv_p', [2, 3, 1024]),
                          ('ssd_p', [2, 512, 128]), ('k_s', [2, 128, 512]), ('v_s', [2, 128, 512]), ('lf_s', [2, 128, 8]),
                          ('s5re_s', [2, 16, 2048]), ('s5im_s', [2, 16, 2048]), ('conv_s', [2, 48, 1024]),
                          ('ssd_s', [2, 16, 512, 128])]:
            OUT[name] = kb.dram(name, shp, F32, kind="ExternalOutput")
        woff = {}
        tot = 0
        for name, cnt, n in WEIGHT_SPECS:
            woff[name] = (tot, n)
            tot += cnt * n
        wscr = kb.dram("wscr", [tot], BF16)
        xs_p = kb.dram("xs_p", [SEQ, D], F32)
        xs_s = kb.dram("xs_s", [128, D], F32)
        kts = kb.dram("kts", [4, 128, SEQ], BF16)
        vss = kb.dram("vss", [4, 128, NKB * 128], BF16)
        s5f = kb.dram("s5f", [2, 4, 128, 1024], F32)
        s5b = kb.dram("s5b", [2, 4, 128, 2048], BF16)

        def wmat(name, l, rows, cols, sub=0):
            off, n = woff[name]
            o = off + l * n + sub
            return wscr.t[o:o + rows * cols].rearrange("(r c) -> r c", c=cols)

        cf = kb.sb("cf", [128, NCA], F32)
        cbt = kb.sb("cbt", [128, NCB], BF16)
        P = [kb.ps("ps", [128, 512], F32) for _ in range(8)]
        acc = P[0:4]
        ppool = P[4:8]
        pidx = [0]

        def pn():
            b = ppool[pidx[0] % 4]
            pidx[0] += 1
            return b

        wbufs = [kb.sb("wbuf", [128, 4096], BF16) for _ in range(NW)]
        par = kb.sb("par", [128, NPAR], F32)
        s5s = kb.sb("s5s", [128, 2, 8, 16], F32)

        def CA(name, w=128, rows=128, off=0):
            c = ca[name] + off
            return cf.t[0:rows, c:c + w]

        def CBF(name, w=128, off=0, rows=128):
            c = cbm[name] + off
            return cbt.t[0:rows, c:c + w]

        def PAR(name, j=0, w=1, rows=128, r0=0):
            c = PC[name] + j
            return par.t[r0:r0 + rows, c:c + w]

        ident_f = CA('ident')
        ident_b = CBF('ident')

        phase_base = [0]

        def ck(i):
            if cfg.stop is not None and cfg.stop == i + phase_base[0]:
                raise _Stop()

        kb.dma('sp', cf.t[:], IN['cstA'])
        with kb.scope() as sc:
            tmp = sc.sb("cstmp", [128, NCB], F32)
            kb.dma('sp', tmp.t[:], IN['cstB'])
            kb.cp('dve', cbt.t[:], tmp.t[:])

        if cfg.stop == 0:
            kb.drain()
            return nc
        with kb.scope() as sc:
            stg = [sc.sb("cvf", [128, 2048], F32) for _ in range(3)]
            stb = [sc.sb("cvb", [128, 2048], BF16) for _ in range(3)]
            ci = 0
            for name, cnt, n in WEIGHT_SPECS:
                tn = cnt * n
                src = IN[name].rearrange("a n -> (a n)").rearrange("(p c) -> p c", p=128)
                o = woff[name][0]
                dst = wscr.t[o:o + tn].rearrange("(p c) -> p c", p=128)
                ncol = tn // 128
                for c0 in range(0, ncol, 2048):
                    cw = min(2048, ncol - c0)
                    a, b = stg[ci % 3], stb[ci % 3]
                    kb.dma('sp', a.t[:, :cw], src[:, c0:c0 + cw])
                    kb.cp('dve' if ci % 2 == 0 else 'act', b.t[:, :cw], a.t[:, :cw])
                    kb.dma('pool', dst[:, c0:c0 + cw], b.t[:, :cw])
                    ci += 1

        def s5_setup(l):
            with kb.scope() as sc:
                lam = sc.sb("lam", [128, 3, 16], F32)
                kb.dma('sp', lam.t[:, 0, :], IN['s5_lam_re'][l].rearrange("(gp q) -> q gp", q=128), allow_slow_non_contiguous=True)
                kb.dma('sp', lam.t[:, 1, :], IN['s5_lam_im'][l].rearrange("(gp q) -> q gp", q=128), allow_slow_non_contiguous=True)
                for g2 in range(2):
                    kb.dma('sp', lam.t[64 * g2:64 * g2 + 64, 2, :],
                           IN['s5_log_dt'][l].rearrange("(gp g) -> g gp", g=2)[g2].partition_broadcast(64), allow_slow_non_contiguous=True)
                w = sc.sb("s5w", [128, 16, 16], F32)
                lr, li = lam.t[:, 0, :], lam.t[:, 1, :]
                W = lambda i: w.t[:, i, :]
                S = lambda i: s5s.t[:, l, i, :]
                kb.act(W(0), lam.t[:, 2, :], AF.Exp)
                kb.tt('dve', W(1), lr, W(0), ALU.mult)
                kb.tt('dve', W(2), li, W(0), ALU.mult)
                kb.act(S(0), W(1), AF.Exp)

                def sincos(outs, outc, ang, shp):
                    kf = sc.sb("rrk", shp, F32)
                    ki = sc.sb("rri", shp, I32)
                    C1, C2 = 6.28125, 2.0 * PI - 6.28125
                    for out, shift in ((outs, 0.0), (outc, 0.5 * PI)):
                        kb.ts('dve', kf.t[:], ang, shift, ALU.add, 1.0 / (2.0 * PI), ALU.mult)
                        kb.cp('dve', ki.t[:], kf.t[:])
                        kb.cp('dve', kf.t[:], ki.t[:])
                        kb.ts('dve', out, ang, shift, ALU.add)
                        kb.stt('dve', out, kf.t[:], -C1, out, ALU.mult, ALU.add)
                        kb.stt('dve', out, kf.t[:], -C2, out, ALU.mult, ALU.add)
                        kb.ts('dve', kf.t[:], out, PI, ALU.is_gt)
                        kb.stt('dve', out, kf.t[:], -2.0 * PI, out, ALU.mult, ALU.add)
                        kb.ts('dve', kf.t[:], out, -PI, ALU.is_lt)
                        kb.stt('dve', out, kf.t[:], 2.0 * PI, out, ALU.mult, ALU.add)
                        kb.ts('dve', out, out, PI, ALU.min, -PI, ALU.max)
                        kb.act(out, out, AF.Sin)

                sincos(W(3), W(4), W(2), [128, 16])
                kb.tt('dve', S(1), S(0), W(4), ALU.mult)
                kb.tt('dve', S(2), S(0), W(3), ALU.mult)
                kb.ts('dve', W(5), W(2), 127.0, ALU.mult)
                sincos(S(4), S(3), W(5), [128, 16])
                kb.ts('dve', W(5), W(2), 7.0, ALU.mult)
                sincos(S(6), S(5), W(5), [128, 16])
                kb.ts('dve', W(5), S(1), -1.0, ALU.add)
                kb.tt('dve', W(6), lr, lr, ALU.mult)
                kb.tt('dve', W(7), li, li, ALU.mult)
                kb.tt('dve', W(6), W(6), W(7), ALU.add)
                kb.op('dve', lambda g: g.reciprocal(out=W(6), in_=W(6)), [W(6)], [W(6)])
                kb.tt('dve', W(7), W(5), lr, ALU.mult)
                kb.tt('dve', W(8), S(2), li, ALU.mult)
                kb.tt('dve', W(7), W(7), W(8), ALU.add)
                kb.tt('dve', W(9), W(7), W(6), ALU.mult)
                kb.tt('dve', W(7), S(2), lr, ALU.mult)
                kb.tt('dve', W(8), W(5), li, ALU.mult)
                kb.tt('dve', W(7), W(7), W(8), ALU.subtract)
                kb.tt('dve', W(10), W(7), W(6), ALU.mult)
                ang = sc.sb("ang", [128, 16, 128], F32)
                ctab = sc.sb("ctab", [128, 16, 128], F32)
                stab = sc.sb("stab", [128, 16, 128], F32)
                kb.tt('dve', ang.t[:], W(2).unsqueeze(2).to_broadcast([128, 16, 128]),
                      CA('iota').unsqueeze(1).to_broadcast([128, 16, 128]), ALU.mult)
                sincos(stab.t[:], ctab.t[:], ang.t[:], [128, 16, 128])
                for g4 in range(4):
                    kb.dma('pool', s5f.t[l, g4, :, 0:512].rearrange("p (g t) -> p g t", g=4), ctab.t[:, 4 * g4:4 * g4 + 4, :])
                    kb.dma('pool', s5f.t[l, g4, :, 512:1024].rearrange("p (g t) -> p g t", g=4), stab.t[:, 4 * g4:4 * g4 + 4, :])
                bn = sc.sb("bn", [128, 2, 16, 16], F32)
                kb.dma('sp', bn.t[:, 0], IN['s5_b_re'][l].rearrange("(gp q) c -> q gp c", q=128))
                kb.dma('sp', bn.t[:, 1], IN['s5_b_im'][l].rearrange("(gp q) c -> q gp c", q=128))
                bb = sc.sb("bb", [128, 4, 16, 16], F32)
                qre_b = W(9).unsqueeze(2).to_broadcast([128, 16, 16])
                qim_b = W(10).unsqueeze(2).to_broadcast([128, 16, 16])
                kb.tt('dve', bb.t[:, 0], bn.t[:, 0], qre_b, ALU.mult)
                kb.tt('dve', bb.t[:, 1], bn.t[:, 1], qim_b, ALU.mult)
                kb.tt('dve', bb.t[:, 0], bb.t[:, 0], bb.t[:, 1], ALU.subtract)
                kb.tt('dve', bb.t[:, 2], bn.t[:, 0], qim_b, ALU.mult)
                kb.tt('dve', bb.t[:, 3], bn.t[:, 1], qre_b, ALU.mult)
                kb.tt('dve', bb.t[:, 2], bb.t[:, 2], bb.t[:, 3], ALU.add)
                nat = [sc.sb("nat", [128, 16, 128], F32) for _ in range(4)]
                for t_ in nat:
                    kb.memset('pool', t_.t[:], 0.0)
                for k_, src_i in ((0, 0), (1, 2)):
                    for g2 in range(2):
                        for j in range(4):
                            lo = 32 * j + 16 * g2
                            kb.cp('dve', nat[k_].t[:].rearrange("p (t j) c -> p t j c", j=4)[64 * g2:64 * g2 + 64, :, j, lo:lo + 16],
                                  bb.t[:, src_i].rearrange("p (t j) c -> p t j c", j=4)[64 * g2:64 * g2 + 64, :, j, :])
                for k_, nm in ((2, 's5_c_re'), (3, 's5_c_im')):
                    for g2 in range(2):
                        for j in range(4):
                            lo = 32 * j + 16 * g2
                            kb.dma('sp', nat[k_].t[:].rearrange("p (t j) m -> p t j m", j=4)[lo:lo + 16, :, j, 64 * g2:64 * g2 + 64],
                                   IN[nm][l].rearrange("(t j g c) n -> j g c t n", t=4, j=4, g=2)[j, g2])
                lb = sc.sb("s5lb", [128, 4, 16, 128], BF16)
                for k_ in range(4):
                    for g4 in range(4):
                        ps = pn()
                        for i in range(4):
                            kb.tr(ps.t[:, i * 128:(i + 1) * 128], nat[k_].t[:, 4 * g4 + i, :], ident_f)
                        dst = lb.t[:, k_, 4 * g4:4 * g4 + 4, :]
                        src = ps.t[:, :].rearrange("p (g m) -> p g m", g=4)
                        if k_ == 3:
                            kb.ts('dve', dst, src, -1.0, ALU.mult)
                        else:
                            kb.cp('act', dst, src)
                for g4 in range(4):
                    kb.dma('pool', s5b.t[l, g4].rearrange("p (k g m) -> p k g m", k=4, g=4), lb.t[:, :, 4 * g4:4 * g4 + 4, :])

        if cfg.stop == 1:
            kb.drain()
            return nc
        for l in range(DEPTH):
            s5_setup(l)
            if cfg.stop == 2:
                kb.drain()
                return nc

        def load_par(l):
            q = 'sp'
            ns = dict(allow_slow_non_contiguous=True)
            kb.dma(q, par.t[:, PC['bg']:PC['bg'] + 24], IN['b_gate'][l].rearrange("(j p) -> p j", p=128), **ns)
            kb.dma(q, par.t[:, PC['s5d']:PC['s5d'] + 4], IN['s5_d'][l].rearrange("(j p) -> p j", p=128), **ns)
            kb.dma(q, par.t[:, PC['bglu']:PC['bglu'] + 4], IN['s5_b_glu'][l].rearrange("(j p) -> p j", p=128), **ns)
            for k_ in range(4):
                kb.dma(q, par.t[:, PC['convw']:PC['convw'] + 32].rearrange("p (t k) -> p t k", k=4)[:, :, k_],
                       IN['ssd_conv_w'][l, k_].rearrange("(t p) -> p t", p=128), **ns)
            kb.dma(q, par.t[:, PC['convb']:PC['convb'] + 8], IN['ssd_conv_b'][l].rearrange("(t p) -> p t", p=128), **ns)
            kb.dma(q, par.t[:, PC['nw']:PC['nw'] + 4], IN['ssd_norm_w'][l].rearrange("(j p) -> p j", p=128), **ns)
            for h in range(8):
                kb.dma(q, par.t[64 * (h % 2):64 * (h % 2) + 64, PC['dcol'] + h // 2:PC['dcol'] + h // 2 + 1],
                       IN['ssd_d'][l, h:h + 1].partition_broadcast(64), **ns)
            kb.dma(q, par.t[:, PC['bfg']:PC['bfg'] + 8], IN['b_fgate'][l].partition_broadcast(128), **ns)
            kb.dma(q, par.t[:, PC['dtb']:PC['dtb'] + 8], IN['ssd_dt_bias'][l].partition_broadcast(128), **ns)
            kb.dma(q, par.t[:, PC['arow']:PC['arow'] + 8], IN['ssd_a_log'][l].partition_broadcast(128), **ns)
            kb.dma(q, par.t[:, PC['brow']:PC['brow'] + 8], IN['moe_b_router'][0].partition_broadcast(128), **ns)
            kb.act(PAR('arow', 0, 8), PAR('arow', 0, 8), AF.Exp)
            kb.ts('dve', PAR('arow', 0, 8), PAR('arow', 0, 8), -1.0, ALU.mult)

        class WS:
            def __init__(s):
                s.plan, s.pos, s.issued, s.views = [], 0, 0, {}

            def add(s, key, src, a, b):
                s.plan.append((key, src, a, b))

            def _issue(s):
                key, src, a, b = s.plan[s.issued]
                buf = wbufs[s.issued % NW]
                v = buf.t[:, 0:a * b].rearrange("p (a b) -> p a b", a=a)
                kb.dma('sp', v, src)
                s.views[s.issued] = v
                s.issued += 1

            def get(s, key):
                assert s.plan[s.pos][0] == key, (s.plan[s.pos][0], key)
                while s.issued < len(s.plan) and s.issued <= s.pos + NW - 2:
                    s._issue()
                v = s.views.pop(s.pos)
                s.pos += 1
                return v

        ws = WS()

        def plan_tile(l, tag):
            win = wmat('w_in', l, D, DIN).rearrange("(kt p) c -> p kt c", p=128)
            A = lambda key, src, a, b: ws.add((tag,) + key, src, a, b)
            A(('u',), win[:, :, C_U:C_U + 512], 8, 512)
            A(('glu',), wmat('s5_w_glu', l, 512, 512).rearrange("(kt p) c -> p kt c", p=128), 4, 512)
            A(('q',), win[:, :, C_Q:C_Q + 512], 8, 512)
            A(('k',), win[:, :, C_K:C_K + 512], 8, 512)
            A(('v',), win[:, :, C_V:C_V + 512], 8, 512)
            A(('f',), win[:, :, C_F:C_F + 8], 8, 8)
            A(('z',), win[:, :, C_Z:C_Z + 512], 8, 512)
            A(('xbc0',), win[:, :, C_XBC:C_XBC + 512], 8, 512)
            A(('xbc1',), win[:, :, C_XBC + 512:C_XBC + 1024], 8, 512)
            A(('dt',), win[:, :, C_DT:C_DT + 8], 8, 8)
            for half in range(2):
                for i, nm in enumerate(('w_branch_s5', 'w_branch_fox', 'w_branch_ssd')):
                    A(('gate', i, half), win[:, :, i * 1024 + half * 512:i * 1024 + half * 512 + 512], 8, 512)
                    A(('br', i, half), wmat(nm, l, 512, D).rearrange("(kt p) c -> p kt c", p=128)[:, :, half * 512:half * 512 + 512], 4, 512)
            for half in range(2):
                A(('wo', half), wmat('w_o', l, D, D).rearrange("(kt p) c -> p kt c", p=128)[:, :, half * 512:half * 512 + 512], 8, 512)
            if l % 2 == 0:
                wg = wmat('ffn_w_gate', l // 2, D, DFF).rearrange("(kt p) c -> p kt c", p=128)
                wu = wmat('ffn_w_up', l // 2, D, DFF).rearrange("(kt p) c -> p kt c", p=128)
                wd = wmat('ffn_w_down', l // 2, DFF, D).rearrange("(fc p) c -> p fc c", p=128)
                for fb in range(6):
                    w_ = min(512, DFF - fb * 512)
                    A(('fg', fb), wg[:, :, fb * 512:fb * 512 + w_], 8, w_)
                    A(('fu', fb), wu[:, :, fb * 512:fb * 512 + w_], 8, w_)
                for half in range(2):
                    for kg in range(3):
                        n_ = min(8, 22 - kg * 8)
                        A(('fd', half, kg), wd[:, kg * 8:kg * 8 + n_, half * 512:half * 512 + 512], n_, 512)
            else:
                A(('rt',), wmat('moe_w_router', l // 2, D, NE).rearrange("(kt p) c -> p kt c", p=128), 8, 8)
                for e in range(NE):
                    wg = wmat('moe_w_gate', l // 2, D, D, sub=e * D * D).rearrange("(kt p) c -> p kt c", p=128)
                    wu = wmat('moe_w_up', l // 2, D, D, sub=e * D * D).rearrange("(kt p) c -> p kt c", p=128)
                    wd = wmat('moe_w_down', l // 2, D, D, sub=e * D * D).rearrange("(fc p) c -> p fc c", p=128)
                    for hf in range(2):
                        A(('mg', e, hf), wg[:, :, hf * 512:hf * 512 + 512], 8, 512)
                        A(('mu', e, hf), wu[:, :, hf * 512:hf * 512 + 512], 8, 512)
                    for half in range(2):
                        A(('md', e, half), wd[:, :, half * 512:half * 512 + 512], 8, 512)

        for l in range(DEPTH):
            for ti in range(NT):
                plan_tile(l, ('P', l, ti))
        for l in range(DEPTH):
            plan_tile(l, ('S', l, 0))

        AX = mybir.AxisListType

        def to_fm(src, dst, NBK):
            for dt_ in range(8):
                ps = pn()
                for blk in range(NBK):
                    kb.tr(ps.t[:, blk * 128:(blk + 1) * 128], src.t[:, blk, dt_ * 128:(dt_ + 1) * 128], ident_f)
                kb.cp('act' if dt_ % 2 else 'dve', dst.t[:, dt_, :], ps.t[:, :NBK * 128])

        def proj_fm(hT, wv, N, nchunk, consume, KT=8):
            for j in range(nchunk):
                ps = pn()
                for kt in range(KT):
                    kb.mm(ps.t[:, :N], wv[:, kt, j * 128:(j + 1) * 128], hT.t[:, kt, :N], start=(kt == 0), stop=(kt == KT - 1))
                consume(j, ps.t[:, :N])

        def proj_tm(hT, wv, ncols, NBK, consume, KT=8):
            for blk in range(NBK):
                ps = pn()
                for kt in range(KT):
                    kb.mm(ps.t[:, :ncols], hT.t[:, kt, blk * 128:(blk + 1) * 128], wv[:, kt, :ncols], start=(kt == 0), stop=(kt == KT - 1))
                consume(blk, ps.t[:, :ncols])

        def layernorm(xt, lnr, NBK, sc):
            st = sc.sb("lnst", [128, 2, 6], F32)
            mv = sc.sb("lnmv", [128, 2], F32)
            rs = sc.sb("lnrs", [128, 1], F32)
            for blk in range(NBK):
                x = xt.t[:, blk, :]
                kb.op('dve', lambda g: g.bn_stats(out=st.t[:, 0, :], in_=xt.t[:, blk, 0:512]), [st.t[:]], [x])
                kb.op('dve', lambda g: g.bn_stats(out=st.t[:, 1, :], in_=xt.t[:, blk, 512:1024]), [st.t[:]], [x])
                kb.op('dve', lambda g: g.bn_aggr(out=mv.t[:], in_=st.t[:]), [mv.t[:]], [st.t[:]])
                kb.ts('dve', rs.t[:], mv.t[:, 1:2], LN_EPS, ALU.add)
                kb.act(rs.t[:], rs.t[:], AF.Sqrt)
                kb.op('dve', lambda g: g.reciprocal(out=rs.t[:], in_=rs.t[:]), [rs.t[:]], [rs.t[:]])
                kb.ts('dve', x, x, mv.t[:, 0:1], ALU.subtract, rs.t[:, 0:1], ALU.mult)
                kb.tt('dve', x, x, lnr.t[:, 0, :], ALU.mult)
                kb.tt('pool', x, x, lnr.t[:, 1, :], ALU.add)

        def softplus_inplace(x, sc, shape):
            a = sc.sb("spa", shape, F32)
            kb.act(a.t[:], x, AF.Abs)
            kb.act(a.t[:], a.t[:], AF.Exp, scale=-1.0)
            kb.act(a.t[:], a.t[:], AF.Ln, bias=1.0)
            kb.ts('dve', x, x, 0.0, ALU.max)
            kb.tt('dve', x, x, a.t[:], ALU.add)

        def logsigmoid_inplace(x, sc, shape):
            a = sc.sb("lsa", shape, F32)
            kb.act(a.t[:], x, AF.Abs)
            kb.act(a.t[:], a.t[:], AF.Exp, scale=-1.0)
            kb.act(a.t[:], a.t[:], AF.Ln, bias=1.0)
            kb.ts('dve', x, x, 0.0, ALU.min)
            kb.tt('dve', x, x, a.t[:], ALU.subtract)

        def recip(out, in_):
            return kb.op('dve', lambda g: g.reciprocal(out=out, in_=in_), [out], [in_])

        def run_layer(l, ph):
            Pm = (ph == 'P')
            TTc = TT if Pm else 128
            NBK = TTc // 128
            ntile = NT if Pm else 1
            NB, CL = (1, 128) if Pm else (16, 8)
            if l == 0:
                xin = IN['xp'] if Pm else IN['xsm']
            else:
                xin = xs_p.t if Pm else xs_s.t
            if l == DEPTH - 1:
                xout = OUT['y_p'] if Pm else OUT['y_s']
            else:
                xout = xs_p if Pm else xs_s
            sfx = '_p' if Pm else '_s'
            load_par(l)
            ck(4)
            lsc_cm = kb.scope()
            lsc = lsc_cm.__enter__()
            ST = lsc.sb("ST", [128, 512], F32)
            STb = lsc.sb("STb", [128, 512], BF16)
            hist = lsc.sb("hist", [128, 8, 3], F32)
            xprev = lsc.sb("xprev", [128, 2, 16, NB], F32)
            ctm = lsc.sb("ctm", [128, 8], F32)
            cfm = lsc.sb("cfm", [128, 1], F32)
            negc = lsc.sb("negc", [128, NKB if Pm else 1, 8], F32)
            rtab = None
            kb.memset('pool', ST.t[:], 0.0)
            kb.memset('pool', STb.t[:], 0.0)
            kb.memset('pool', hist.t[:], 0.0)
            kb.memset('pool', ctm.t[:], 0.0)
            kb.memset('pool', cfm.t[:], 0.0)
            if Pm:
                kb.memset('pool', xprev.t[:], 0.0)
            else:
                rtab = lsc.sb("rtab", [128, 16, 128], F32)
                kb.tt('dve', rtab.t[:], s5s.t[:, l, 0, :].unsqueeze(2).to_broadcast([128, 16, 128]),
                      CA('notfirst').unsqueeze(1).to_broadcast([128, 16, 128]), ALU.mult)
                with kb.scope() as sc:
                    st0 = sc.sb("st0", [16, 2, 2048], F32)
                    kb.dma('sp', st0.t[:, 0, :], IN['s5re0'][l])
                    kb.dma('sp', st0.t[:, 1, :], IN['s5im0'][l])
                    for comp in range(2):
                        ps = pn()
                        for gp in range(16):
                            kb.tr(ps.t[:, gp * 16:(gp + 1) * 16], st0.t[:, comp, gp * 128:(gp + 1) * 128], CA('ident', 16, 16))
                        kb.cp('act', xprev.t[:, comp], ps.t[:, 0:256].rearrange("p (g b) -> p g b", g=16))

            def tile(ti):
                tag = (ph, l, ti)
                t0 = ti * TTc
                last = (ti == ntile - 1)

                def G(*k):
                    return ws.get((tag,) + k)

                tsc_cm = kb.scope()
                tsc = tsc_cm.__enter__()
                xt = tsc.sb("xt", [128, NBK, D], F32)
                hT = tsc.sb("hT", [128, 8, TTc], BF16)
                yT = [tsc.sb("yT%d" % i, [128, 4, TTc], BF16) for i in range(3)]
                kb.dma('pool', xt.t[:], xin[t0:t0 + TTc, :].rearrange("(b p) d -> p b d", p=128))
                to_fm(xt, hT, NBK)
                ck(5)

                with kb.scope() as sc:
                    uT = sc.sb("uT", [128, 4, TTc], BF16)
                    wu = G('u')
                    proj_fm(hT, wu, TTc, 4, lambda j, p: kb.cp('act', uT.t[:, j, :], p))
                    ypre = sc.sb("ypre", [128, 4, TTc], F32)
                    bur, bui, t1, t2, t3, t4, zr, zi, Zr, Zi = [sc.sb(n_, [128, 4, 128], F32) for n_ in
                                                                ("bur", "bui", "t1", "t2", "t3", "t4", "zr", "zi", "Zr", "Zi")]
                    Xr = sc.sb("Xr", [128, 4, 128], BF16)
                    Xi = sc.sb("Xi", [128, 4, 128], BF16)
                    zi0 = sc.sb("zi0", [128, 2, 4, NB], F32)
                    tmpc = sc.sb("tmpc", [128, 4, 4, NB], F32)
                    tabf = [sc.sb("tabf", [128, 2, 4, 128], F32) for _ in range(2)]
                    tabb = [sc.sb("tabb", [128, 4, 4, 128], BF16) for _ in range(2)]
                    if Pm:
                        V = lambda x: x.t[:]
                    else:
                        V = lambda x: x.t[:].rearrange("p g (b t) -> p g b t", t=8)
                    VS = lambda x: x.t[:].rearrange("p g (b t) -> p g b t", t=CL)
                    for g4 in range(4):
                        tf, tb = tabf[g4 % 2], tabb[g4 % 2]
                        kb.dma('sp', tf.t[:], s5f.t[l, g4].rearrange("p (k g t) -> p k g t", k=2, g=4))
                        kb.dma('sp', tb.t[:], s5b.t[l, g4].rearrange("p (k g m) -> p k g m", k=4, g=4))
                        if Pm:
                            cosv, sinv = tf.t[:, 0], tf.t[:, 1]
                        else:
                            cosv = tf.t[:, 0, :, 0:8].unsqueeze(2).to_broadcast([128, 4, 16, 8])
                            sinv = tf.t[:, 1, :, 0:8].unsqueeze(2).to_broadcast([128, 4, 16, 8])
                        gs = slice(4 * g4, 4 * g4 + 4)
                        are = s5s.t[:, l, 1, gs].unsqueeze(2).to_broadcast([128, 4, NB])
                        aim = s5s.t[:, l, 2, gs].unsqueeze(2).to_broadcast([128, 4, NB])
                        cL = s5s.t[:, l, 3 if Pm else 5, gs].unsqueeze(2).to_broadcast([128, 4, NB])
                        sL = s5s.t[:, l, 4 if Pm else 6, gs].unsqueeze(2).to_broadcast([128, 4, NB])
                        Y = acc[g4 % 2]
                        for c in range(NBK):
                            tok = slice(c * 128, (c + 1) * 128)
                            psr, psi = pn(), pn()
                            for i in range(4):
                                kb.mm(psr.t[:, i * 128:(i + 1) * 128], tb.t[:, 0, i, :], uT.t[:, g4, tok])
                                kb.mm(psi.t[:, i * 128:(i + 1) * 128], tb.t[:, 1, i, :], uT.t[:, g4, tok])
                            kb.cp('act', bur.t[:], psr.t[:, :].rearrange("p (g t) -> p g t", g=4))
                            kb.cp('act', bui.t[:], psi.t[:, :].rearrange("p (g t) -> p g t", g=4))
                            kb.tt('dve', V(t1), V(bur), cosv, ALU.mult)
                            kb.tt('dve', V(t2), V(bui), sinv, ALU.mult)
                            kb.tt('dve', zr.t[:], t1.t[:], t2.t[:], ALU.add)
                            kb.tt('pool', V(t3), V(bui), cosv, ALU.mult)
                            kb.tt('pool', V(t4), V(bur), sinv, ALU.mult)
                            kb.tt('pool', zi.t[:], t3.t[:], t4.t[:], ALU.subtract)
                            xr, xi = xprev.t[:, 0, gs, :], xprev.t[:, 1, gs, :]
                            kb.tt('dve', tmpc.t[:, 0], xr, are, ALU.mult)
                            kb.tt('dve', tmpc.t[:, 1], xi, aim, ALU.mult)
                            kb.tt('dve', zi0.t[:, 0], tmpc.t[:, 0], tmpc.t[:, 1], ALU.subtract)
                            kb.tt('dve', tmpc.t[:, 2], xr, aim, ALU.mult)
                            kb.tt('dve', tmpc.t[:, 3], xi, are, ALU.mult)
                            kb.tt('dve', zi0.t[:, 1], tmpc.t[:, 2], tmpc.t[:, 3], ALU.add)
                            kb.tt('dve', VS(zr)[:, :, :, 0], VS(zr)[:, :, :, 0], zi0.t[:, 0], ALU.add)
                            kb.tt('dve', VS(zi)[:, :, :, 0], VS(zi)[:, :, :, 0], zi0.t[:, 1], ALU.add)
                            for i in range(4):
                                gp = 4 * g4 + i
                                d0 = s5s.t[:, l, 0, gp:gp + 1].to_broadcast([128, 128]) if Pm else rtab.t[:, gp, :]
                                kb.scan('dve', Zr.t[:, i, :], d0, zr.t[:, i, :], 0.0)
                                kb.scan('dve', Zi.t[:, i, :], d0, zi.t[:, i, :], 0.0)
                            kb.tt('dve', V(t1), V(Zr), cosv, ALU.mult)
                            kb.tt('dve', V(t2), V(Zi), sinv, ALU.mult)
                            kb.tt('dve', Xr.t[:], t1.t[:], t2.t[:], ALU.subtract)
                            kb.tt('pool', V(t3), V(Zr), sinv, ALU.mult)
                            kb.tt('pool', V(t4), V(Zi), cosv, ALU.mult)
                            kb.tt('pool', Xi.t[:], t3.t[:], t4.t[:], ALU.add)
                            zrl, zil = VS(Zr)[:, :, :, CL - 1], VS(Zi)[:, :, :, CL - 1]
                            kb.tt('dve', tmpc.t[:, 0], zrl, cL, ALU.mult)
                            kb.tt('dve', tmpc.t[:, 1], zil, sL, ALU.mult)
                            kb.tt('dve', xr, tmpc.t[:, 0], tmpc.t[:, 1], ALU.subtract)
                            kb.tt('dve', tmpc.t[:, 2], zrl, sL, ALU.mult)
                            kb.tt('dve', tmpc.t[:, 3], zil, cL, ALU.mult)
                            kb.tt('dve', xi, tmpc.t[:, 2], tmpc.t[:, 3], ALU.add)
                            for i in range(4):
                                kb.mm(Y.t[:, tok], tb.t[:, 2, i, :], Xr.t[:, i, :], start=(i == 0), stop=False)
                                kb.mm(Y.t[:, tok], tb.t[:, 3, i, :], Xi.t[:, i, :], start=False, stop=(i == 3))
                        kb.stt('dve', ypre.t[:, g4, :], uT.t[:, g4, :], PAR('s5d', g4), Y.t[:, :TTc], ALU.mult, ALU.add)
                    ygb = sc.sb("ygb", [128, 4, TTc], BF16)
                    sq = [sc.sb("sq", [128, TTc], F32) for _ in range(2)]
                    for ct in range(4):
                        x = ypre.t[:, ct, :]
                        s_ = sq[ct % 2].t[:]
                        kb.act(s_, x, AF.Square)
                        kb.ts('dve', s_, s_, 0.044715, ALU.mult, 1.0, ALU.add)
                        kb.tt('dve', s_, s_, x, ALU.mult)
                        kb.act(s_, s_, AF.Sigmoid, scale=1.5957691216057308)
                        kb.tt('dve', x, x, s_, ALU.mult)
                        kb.cp('pool', ygb.t[:, ct, :], x)
                    wg = G('glu')

                    def glu_c(j, p):
                        s_ = sq[j % 2].t[:]
                        kb.act(s_, p, AF.Sigmoid, bias=PAR('bglu', j))
                        kb.tt('dve', yT[0].t[:, j, :], ypre.t[:, j, :], s_, ALU.mult)
                    proj_fm(ygb, wg, TTc, 4, glu_c, KT=4)
                    if last:
                        if Pm:
                            kb.dma('pool', OUT['s5re_p'].t[l].rearrange("(gp q) -> q gp", q=128), xprev.t[:, 0, :, 0], allow_slow_non_contiguous=True)
                            kb.dma('pool', OUT['s5im_p'].t[l].rearrange("(gp q) -> q gp", q=128), xprev.t[:, 1, :, 0], allow_slow_non_contiguous=True)
                        else:
                            so = sc.sb("s5o", [16, 2, 2048], F32)
                            for comp, nm in ((0, 's5re_s'), (1, 's5im_s')):
                                for g4 in range(4):
                                    ps = pn()
                                    for i in range(4):
                                        kb.tr(ps.t[:16, i * 128:(i + 1) * 128], xprev.t[:, comp, 4 * g4 + i, :], ident_f)
                                    kb.cp('act', so.t[:, comp, g4 * 512:(g4 + 1) * 512], ps.t[:16, :])
                                kb.dma('pool', OUT[nm].t[l], so.t[:, comp, :])

                ck(6)
                with kb.scope() as sc:
                    qT = sc.sb("qT", [128, 4, TTc], BF16)
                    knT = sc.sb("knT", [128, 4, TTc], BF16)
                    ktm = sc.sb("ktm", [128, NBK, 512], F32)
                    vtm = sc.sb("vtm", [128, NBK, 512], F32)
                    vbn = sc.sb("vbn", [128, NBK, 512], BF16)
                    lf = sc.sb("lf", [128, NBK, 8], F32)
                    wq = G('q')
                    proj_fm(hT, wq, TTc, 4, lambda j, p: kb.ts('dve', qT.t[:, j, :], p, 0.125, ALU.mult))
                    ck(6.01)
                    wk = G('k')
                    proj_fm(hT, wk, TTc, 4, lambda j, p: kb.cp('act', knT.t[:, j, :], p))
                    ck(6.02)
                    proj_tm(hT, wk, 512, NBK, lambda b, p: kb.cp('act', ktm.t[:, b, :], p))
                    ck(6.03)
                    wv = G('v')

                    def v_c(b, p):
                        kb.cp('act', vtm.t[:, b, :], p)
                        kb.cp('dve', vbn.t[:, b, :], vtm.t[:, b, :])
                    proj_tm(hT, wv, 512, NBK, v_c)
                    ck(6.04)
                    wf = G('f')
                    proj_tm(hT, wf, 8, NBK, lambda b, p: kb.tt('dve', lf.t[:, b, :], p, PAR('bfg', 0, 8), ALU.add))
                    ck(6.1)
                    logsigmoid_inplace(lf.t[:], sc, [128, NBK, 8])
                    ck(6.2)
                    kb.dma('pool', OUT['k' + sfx].t[l, t0:t0 + TTc, :].rearrange("(b p) c -> p b c", p=128), ktm.t[:])
                    kb.dma('pool', OUT['v' + sfx].t[l, t0:t0 + TTc, :].rearrange("(b p) c -> p b c", p=128), vtm.t[:])
                    kb.dma('pool', OUT['lf' + sfx].t[l, t0:t0 + TTc, :].rearrange("(b p) c -> p b c", p=128), lf.t[:], allow_slow_non_contiguous=True)
                    ck(6.3)
                    if Pm:
                        fox_prompt(sc, l, ti, TTc, NBK, t0, qT, knT, vbn, lf, negc, ctm, cfm, yT[1])
                    else:
                        fox_sample(sc, l, qT, knT, vbn, lf, yT[1])

                ck(7)
                with kb.scope() as sc:
                    ssd_stage(sc, l, Pm, TTc, NBK, NB, G, hT, yT[2], ST, STb, hist, last)

                ck(8)
                with kb.scope() as sc:
                    mT = sc.sb("mT", [128, 8, TTc], BF16)
                    mfull = sc.sb("mfull", [128, 4, TTc], F32)
                    gt = [sc.sb("gt", [128, TTc], F32) for _ in range(2)]
                    lnr = sc.sb("lnr", [128, 2, D], F32)
                    kb.dma('sp', lnr.t[:, 0, :], IN['ln1_g'][l].partition_broadcast(128))
                    kb.dma('sp', lnr.t[:, 1, :], IN['ln1_b'][l].partition_broadcast(128))
                    gi = 0
                    for half in range(2):
                        for i in range(3):
                            wgt = G('gate', i, half)
                            wbr = G('br', i, half)
                            for oc in range(4):
                                psg = pn()
                                for kt in range(8):
                                    kb.mm(psg.t[:, :TTc], wgt[:, kt, oc * 128:(oc + 1) * 128], hT.t[:, kt, :], start=(kt == 0), stop=(kt == 7))
                                g = gt[gi % 2]
                                gi += 1
                                kb.act(g.t[:], psg.t[:, :TTc], AF.Sigmoid, bias=PAR('bg', i * 8 + half * 4 + oc))
                                psb = pn()
                                for kt in range(4):
                                    kb.mm(psb.t[:, :TTc], wbr[:, kt, oc * 128:(oc + 1) * 128], yT[i].t[:, kt, :], start=(kt == 0), stop=(kt == 3))
                                if i == 0:
                                    kb.tt('dve', mfull.t[:, oc, :], psb.t[:, :TTc], g.t[:], ALU.mult)
                                else:
                                    kb.tt('dve', g.t[:], psb.t[:, :TTc], g.t[:], ALU.mult)
                                    kb.tt('pool', mfull.t[:, oc, :], mfull.t[:, oc, :], g.t[:], ALU.add)
                        kb.cp('act', mT.t[:, 4 * half:4 * half + 4, :], mfull.t[:])
                    for half in range(2):
                        wo = G('wo', half)
                        for blk in range(NBK):
                            ps = pn()
                            for kt in range(8):
                                kb.mm(ps.t[:, :512], mT.t[:, kt, blk * 128:(blk + 1) * 128], wo[:, kt, :], start=(kt == 0), stop=(kt == 7))
                            xs_ = xt.t[:, blk, half * 512:(half + 1) * 512]
                            kb.stt('dve', xs_, xs_, ALPHA, ps.t[:, :512], ALU.mult, ALU.add)
                    layernorm(xt, lnr, NBK, sc)
                    to_fm(xt, hT, NBK)

                ck(9)
                with kb.scope() as sc:
                    lnr = sc.sb("lnr2", [128, 2, D], F32)
                    kb.dma('sp', lnr.t[:, 0, :], IN['ln2_g'][l].partition_broadcast(128))
                    kb.dma('sp', lnr.t[:, 1, :], IN['ln2_b'][l].partition_broadcast(128))
                    sg = [sc.sb("sg", [128, TTc], F32) for _ in range(2)]
                    si = [0]

                    def gate_up(wg_, wu_, nch, hid, fc0):
                        for j in range(nch):
                            psg, psu = pn(), pn()
                            for kt in range(8):
                                kb.mm(psg.t[:, :TTc], wg_[:, kt, j * 128:(j + 1) * 128], hT.t[:, kt, :], start=(kt == 0), stop=(kt == 7))
                            for kt in range(8):
                                kb.mm(psu.t[:, :TTc], wu_[:, kt, j * 128:(j + 1) * 128], hT.t[:, kt, :], start=(kt == 0), stop=(kt == 7))
                            s_ = sg[si[0] % 2]
                            si[0] += 1
                            kb.act(s_.t[:], psg.t[:, :TTc], AF.Silu)
                            kb.tt('dve', hid.t[:, fc0 + j, :], s_.t[:], psu.t[:, :TTc], ALU.mult)

                    if l % 2 == 0:
                        hid = sc.sb("hid", [128, 22, TTc], BF16)
                        for fb in range(6):
                            wg_ = G('fg', fb)
                            wu_ = G('fu', fb)
                            gate_up(wg_, wu_, min(4, 22 - fb * 4), hid, fb * 4)
                        for half in range(2):
                            for kg in range(3):
                                wd = G('fd', half, kg)
                                n_ = min(8, 22 - kg * 8)
                                for blk in range(NBK):
                                    for i in range(n_):
                                        kb.mm(acc[blk].t[:, :512], hid.t[:, kg * 8 + i, blk * 128:(blk + 1) * 128], wd[:, i, :],
                                              start=(kg == 0 and i == 0), stop=(kg == 2 and i == n_ - 1))
                            for blk in range(NBK):
                                xs_ = xt.t[:, blk, half * 512:(half + 1) * 512]
                                kb.stt('dve', xs_, xs_, ALPHA, acc[blk].t[:, :512], ALU.mult, ALU.add)
                    else:
                        wr = G('rt')
                        lg = sc.sb("lg", [128, NBK, 8], F32)
                        gate = sc.sb("gate", [128, NBK, 8], F32)
                        w8 = sc.sb("w8", [128, 4, 8], F32)
                        m = sc.sb("m", [128, 4], F32)
                        proj_tm(hT, wr, 8, NBK, lambda b, p: kb.tt('dve', lg.t[:, b, :], p, PAR('brow', 0, 8), ALU.add))
                        for blk in range(NBK):
                            L_ = lg.t[:, blk, :]
                            kb.op('dve', lambda g: g.reduce_max(out=m.t[:, 0:1], in_=L_, axis=AX.X), [m.t[:]], [L_])
                            kb.ts('dve', w8.t[:, 0, :], L_, m.t[:, 0:1], ALU.is_equal)
                            kb.stt('dve', w8.t[:, 1, :], w8.t[:, 0, :], -1e30, L_, ALU.mult, ALU.add)
                            kb.op('dve', lambda g: g.reduce_max(out=m.t[:, 1:2], in_=w8.t[:, 1, :], axis=AX.X), [m.t[:]], [w8.t[:]])
                            kb.ts('dve', w8.t[:, 2, :], L_, m.t[:, 1:2], ALU.is_ge)
                            kb.ts('dve', m.t[:, 2:3], m.t[:, 0:1], -1.0, ALU.mult)
                            kb.act(w8.t[:, 3, :], L_, AF.Exp, bias=m.t[:, 2:3])
                            kb.tt('dve', w8.t[:, 3, :], w8.t[:, 3, :], w8.t[:, 2, :], ALU.mult)
                            kb.op('dve', lambda g: g.reduce_sum(out=m.t[:, 3:4], in_=w8.t[:, 3, :], axis=AX.X), [m.t[:]], [w8.t[:]])
                            recip(m.t[:, 3:4], m.t[:, 3:4])
                            kb.ts('dve', gate.t[:, blk, :], w8.t[:, 3, :], m.t[:, 3:4], ALU.mult)
                            kb.ts('dve', xt.t[:, blk, :], xt.t[:, blk, :], ALPHA, ALU.mult)
                        hids = [sc.sb("hide", [128, 8, TTc], BF16) for _ in range(2)]
                        for e in range(NE):
                            hid = hids[e % 2]
                            for hf in range(2):
                                wg_ = G('mg', e, hf)
                                wu_ = G('mu', e, hf)
                                gate_up(wg_, wu_, 4, hid, hf * 4)
                            for half in range(2):
                                wd = G('md', e, half)
                                for blk in range(NBK):
                                    ps = pn()
                                    for i in range(8):
                                        kb.mm(ps.t[:, :512], hid.t[:, i, blk * 128:(blk + 1) * 128], wd[:, i, :], start=(i == 0), stop=(i == 7))
                                    xs_ = xt.t[:, blk, half * 512:(half + 1) * 512]
                                    kb.stt('dve', xs_, ps.t[:, :512], gate.t[:, blk, e:e + 1], xs_, ALU.mult, ALU.add)
                    layernorm(xt, lnr, NBK, sc)
                    kb.dma('pool', xout.t[t0:t0 + TTc, :].rearrange("(b p) d -> p b d", p=128), xt.t[:])
                tsc_cm.__exit__(None, None, None)
                ck(10)

            for ti in range(ntile):
                tile(ti)
            lsc_cm.__exit__(None, None, None)

        def fox_prompt(sc, l, ti, TTc, NBK, t0, qT, knT, vbn, lf, negc, ctm, cfm, yout):
            nk = (ti + 1) * TTc
            nkb = nk // 128
            lfrep = sc.sb("lfrep", [128, 96], F32)
            crep = sc.sb("crep", [96, TTc], F32)
            r1 = sc.sb("cr1", [96, TTc], F32)
            l1 = sc.sb("cl1", [96, TTc], BF16)
            cq3 = sc.sb("cq3", [96, TTc], BF16)
            kb.memset('pool', lfrep.t[:], 0.0)
            tri = CA('tri_p')
            for b in range(NBK):
                kbg = ti * NBK + b
                for r0 in (0, 32, 64):
                    kb.cp('dve', lfrep.t[:, r0:r0 + 8], lf.t[:, b, :])
                ps = pn()
                kb.mm(ps.t[:, 0:8], tri, lf.t[:, b, :])
                kb.mm(ps.t[:96, 128:256], lfrep.t[:, 0:96], tri)
                kb.mm(ps.t[:, 8:16], CA('ones'), lf.t[:, b, :])
                kb.mm(ps.t[:96, 16:17], lfrep.t[:, 0:96], CA('ones', 1))
                kb.stt('dve', negc.t[:, kbg, :], ps.t[:, 0:8], -1.0, ctm.t[:], ALU.mult, ALU.subtract)
                kb.ts('dve', crep.t[:, b * 128:(b + 1) * 128], ps.t[:96, 128:256], cfm.t[0:96, 0:1], ALU.add)
                kb.tt('dve', ctm.t[:], ctm.t[:], ps.t[:, 8:16], ALU.add)
                kb.tt('dve', cfm.t[0:96, :], cfm.t[0:96, :], ps.t[:96, 16:17], ALU.add)
            kb.cp('dve', cq3.t[:], crep.t[:])
            kb.tt('dve', r1.t[:], crep.t[:], cq3.t[:], ALU.subtract)
            kb.cp('dve', l1.t[:], r1.t[:])
            kb.tt('dve', r1.t[:], r1.t[:], l1.t[:], ALU.subtract)
            kb.cp('dve', cq3.t[32:40, :], l1.t[32:40, :])
            kb.cp('dve', cq3.t[64:72, :], r1.t[64:72, :])
            ck(6.4)
            kb.dma('pool', kts.t[:, :, t0:t0 + TTc].rearrange("h p t -> p h t"), knT.t[:])
            for b in range(NBK):
                kb.dma('pool', vss.t[:, :, t0 + b * 128:t0 + (b + 1) * 128].rearrange("h p c -> p h c"),
                       vbn.t[:, b, :].rearrange("p (h c) -> p h c", c=128))
            ktb = [sc.sb("ktb", [128, SEQ], BF16) for _ in range(2)]
            vbb = [sc.sb("vbb", [128, NKB, 128], BF16) for _ in range(2)]
            PT = [sc.sb("PT", [128, TTc], BF16) for _ in range(3)]
            rec = sc.sb("rec", [128, TTc], F32)

            def loadkv(hp):
                kb.dma('sp', ktb[hp % 2].t[:, :nk], kts.t[hp, :, :nk])
                kb.dma('sp', vbb[hp % 2].t[:, :nkb, :], vss.t[hp, :, :nk].rearrange("p (b c) -> p b c", c=128))

            loadkv(0)
            ck(6.5)
            pti = 0
            for hp in range(4):
                if hp + 1 < 4:
                    loadkv(hp + 1)
                num, den = acc[2 * (hp % 2)], acc[2 * (hp % 2) + 1]
                kt_, vb_ = ktb[hp % 2], vbb[hp % 2]
                for h2 in range(2):
                    h = 2 * hp + h2
                    po = 64 * h2

                    def score(kbi):
                        S = pn()
                        diag = kbi >= nkb - NBK
                        kb.mm(S.t[:, :TTc], kt_.t[po:po + 64, kbi * 128:(kbi + 1) * 128], qT.t[po:po + 64, hp, :], start=True, stop=False)
                        kb.mm(S.t[:, :TTc], CBF('selh', 128, h * 128, rows=96), cq3.t[0:96, :], start=False, stop=not diag)
                        if diag:
                            j = kbi - (nkb - NBK)
                            kb.mm(S.t[:, :TTc], ident_b, CBF('maskdiag', TTc, j * 512), start=False, stop=True)
                        return S

                    Snext = score(0)
                    for kbi in range(nkb):
                        S = Snext
                        if kbi + 1 < nkb:
                            Snext = score(kbi + 1)
                        pt = PT[pti % 3]
                        pti += 1
                        kb.act(pt.t[:], S.t[:, :TTc], AF.Exp, bias=negc.t[:, kbi, h:h + 1])
                        kb.mm(num.t[po:po + 64, :TTc], vb_.t[:, kbi, po:po + 64], pt.t[:], start=(kbi == 0), stop=(kbi == nkb - 1))
                        kb.mm(den.t[po:po + 64, :TTc], CBF('ones', 64), pt.t[:], start=(kbi == 0), stop=(kbi == nkb - 1))
                recip(rec.t[:], den.t[:, :TTc])
                kb.tt('dve', yout.t[:, hp, :], num.t[:, :TTc], rec.t[:], ALU.mult)
                ck(6.6)

        def fox_sample(sc, l, qT, knT, vbn, lf, yout):
            num, den, numN, denN = acc[0], acc[1], acc[2], acc[3]
            decT = sc.sb("decT", [128, 8, PGT], F32)
            for pt_ in range(nPT):
                rows = min(128, PGT - 128 * pt_)
                idxl = sc.sb("idxl", [128, 1], I32)
                kb.dma('sp', idxl.t[:rows, :], IN['ptab'][0, pt_ * 128:pt_ * 128 + rows].rearrange("(p o) -> p o", o=1))
                lfg = sc.sb("lfg", [128, 1024], F32)
                kb.gather(lfg.t[:rows, :], IN['clf'][l], idxl.t[:rows, 0:1])
                pre = sc.sb("pre", [128, 1024], F32)
                lv = lfg.t[:rows, :].rearrange("p (t h) -> p h t", h=8)
                pv = pre.t[:rows, :].rearrange("p (t h) -> p h t", h=8)
                for h in range(8):
                    kb.scan('dve', pv[:, h, :], CA('ones', 128, rows), lv[:, h, :], 0.0)
                tot = sc.sb("ltot", [128, 8], F32)
                kb.cp('dve', tot.t[:rows, :], pv[:, :, 127])
                ps = pn()
                kb.mm(ps.t[:rows, 0:8], CA('subd', rows, rows), tot.t[:rows, :])
                kb.tt('dve', tot.t[:rows, :], tot.t[:rows, :], ps.t[:rows, 0:8], ALU.add)
                dec = sc.sb("dec", [128, 8, 128], F32)
                kb.tt('dve', dec.t[:rows], tot.t[:rows, :].unsqueeze(2).to_broadcast([rows, 8, 128]), pv, ALU.subtract)
                for h in range(8):
                    ps = pn()
                    kb.tr(ps.t[:, 0:rows], dec.t[:rows, h, :], CA('ident', rows, rows))
                    kb.cp('act', decT.t[:, h, pt_ * 128:pt_ * 128 + rows], ps.t[:, 0:rows])
            ck(6.71)
            ptb = sc.sb("ptb", [128, PGT], I32)
            ptf = sc.sb("ptf", [128, PGT], F32)
            idx = sc.sb("idx", [128, PGT], I32)
            kb.dma('sp', ptb.t[:], IN['ptab'][0].partition_broadcast(128))
            kb.cp('dve', ptf.t[:], ptb.t[:])
            kb.ts('dve', idx.t[:], ptf.t[:], 128.0, ALU.mult, CA('piota', 1), ALU.add)
            ck(6.72)
            accs = [acc[0], acc[1]]
            zl = sc.sb("zl", [128, 128], BF16)
            kb.memset('pool', zl.t[:], 0.0)
            for k_ in range(2):
                kb.mm(accs[k_].t[:, :], zl.t[:], CBF('maskdiag', 512), start=True, stop=False)
            HS = lambda h: slice((h % 4) * 65, (h % 4) * 65 + 65)
            qbd = sc.sb("qbd", [128, 4, 16, 16], BF16)
            kb.memset('pool', qbd.t[:], 0.0)
            kb.cp('dve', qbd.t[0:64, :, :, 0:8], qT.t[0:64, :, :].rearrange("p c (b q) -> p c b q", q=8))
            kb.cp('dve', qbd.t[64:128, :, :, 8:16], qT.t[64:128, :, :].rearrange("p c (b q) -> p c b q", q=8))
            cn = sc.sb("cn", [128, 8], F32)
            ps = pn()
            kb.mm(ps.t[:, 0:8], CA('tri_s'), lf.t[:, 0, :])
            kb.ts('dve', cn.t[:], ps.t[:, 0:8], -1.0, ALU.mult)
            vn65 = sc.sb("vn65", [128, 8, 65], BF16)
            kb.memset('pool', vn65.t[:, :, 64:65], 1.0)
            kb.cp('dve', vn65.t[:, :, 0:64], vbn.t[:, 0, :].rearrange("p (h d) -> p h d", d=64))
            PN = [sc.sb("PN", [128, 128], BF16) for _ in range(2)]
            for h in range(8):
                hp, po = h // 2, 64 * (h % 2)
                S = pn()
                kb.mm(S.t[:, 0:128], knT.t[po:po + 64, hp, :], qT.t[po:po + 64, hp, :], start=True, stop=False)
                kb.mm(S.t[:, 0:128], ident_b, CBF('masknew'), start=False, stop=True)
                pnw = PN[h % 2]
                kb.act(pnw.t[:], S.t[:, 0:128], AF.Exp, bias=cn.t[:, h:h + 1])
                kb.mm(accs[h // 4].t[:, HS(h)], pnw.t[:], vn65.t[:, h, :], start=False, stop=False)
            ck(6.73)
            PGH = min(8, NPG)
            kst = [sc.sb("kst", [128, 512], F32) for _ in range(3)]
            kbf = sc.sb("kbf", [128, 512], BF16)
            KTb = [sc.sb("KTb", [128, 4, NPG * 128], BF16) for _ in range(2)]
            Vb = [sc.sb("Vb", [128, NPG, 8, 65], BF16) for _ in range(2)]
            for v_ in Vb:
                kb.memset('pool', v_.t[:, :, :, 64:65], 1.0)
            sbs = [sc.sb("sbs", [128, 512], F32) for _ in range(2)]
            PTp = sc.sb("PTp", [128, PGH, 8, 128], BF16)
            kb.memset('pool', PTp.t[:], 0.0)
            ki = 0
            ckr, cvr = IN['ck'][l], IN['cv'][l]
            W_ = PGH * 64
            for b in range(16):
                ktb_, vb_ = KTb[b % 2], Vb[b % 2]
                for pg in range(NPG):
                    e = b * NPG + pg
                    a_ = kst[ki % 3]
                    kb.gather(a_.t[:], ckr, idx.t[:, e:e + 1])
                    kb.cp('dve', kbf.t[:], a_.t[:])
                    ps = pn()
                    pb = ps.t[:, :].bitcast(BF16)
                    for c4 in range(4):
                        kb.tr(pb[:, c4 * 128:(c4 + 1) * 128], kbf.t[:, c4 * 128:(c4 + 1) * 128], ident_b)
                    kb.cp('act', ktb_.t[:, :, pg * 128:(pg + 1) * 128], pb[:, 0:512].rearrange("p (c t) -> p c t", c=4))
                    ki += 1
                    a_ = kst[ki % 3]
                    kb.gather(a_.t[:], cvr, idx.t[:, e:e + 1])
                    kb.cp('dve', vb_.t[:, pg, :, 0:64], a_.t[:].rearrange("p (h d) -> p h d", d=64))
                    ki += 1
                for gi, pg0 in enumerate(range(0, NPG, PGH)):
                    S = pn()
                    for pg in range(pg0, pg0 + PGH):
                        for hp in range(4):
                            c0 = ((pg - pg0) * 4 + hp) * 16
                            kb.mm(S.t[:, c0:c0 + 16], ktb_.t[:, hp, pg * 128:(pg + 1) * 128], qbd.t[:, hp, b, :])
                    s_ = sbs[gi % 2]
                    dv = decT.t[:, :, b * NPG + pg0:b * NPG + pg0 + PGH].rearrange("p h g -> p g h").unsqueeze(3).to_broadcast([128, PGH, 8, 8])
                    kb.tt('dve', s_.t[:, :W_].rearrange("p (g h q) -> p g h q", g=PGH, h=8), S.t[:, :W_].rearrange("p (g h q) -> p g h q", g=PGH, h=8), dv, ALU.add)
                    kb.act(PTp.t[:, :, :, b * 8:(b + 1) * 8], s_.t[:, :W_].rearrange("p (g h q) -> p g h q", g=PGH, h=8), AF.Exp)
                    for pg in range(pg0, pg0 + PGH):
                        for h in range(8):
                            kb.mm(accs[h // 4].t[:, HS(h)], PTp.t[:, pg - pg0, h, :], vb_.t[:, pg, h, :], start=False, stop=False)
                    kb.memset('pool', PTp.t[:, :, :, b * 8:(b + 1) * 8], 0.0)
            for k_ in range(2):
                kb.mm(accs[k_].t[:, :], zl.t[:], CBF('maskdiag', 512), start=False, stop=True)
            rec8 = sc.sb("rec8", [128, 8], F32)
            ytm = sc.sb("ytm", [128, 8, 64], F32)
            for k_ in range(2):
                av = accs[k_].t[:, 0:260].rearrange("p (h c) -> p h c", c=65)
                kb.cp('dve', rec8.t[:, 4 * k_:4 * k_ + 4], av[:, :, 64])
            recip(rec8.t[:], rec8.t[:])
            for k_ in range(2):
                av = accs[k_].t[:, 0:260].rearrange("p (h c) -> p h c", c=65)
                kb.tt('dve', ytm.t[:, 4 * k_:4 * k_ + 4, :], av[:, :, 0:64], rec8.t[:, 4 * k_:4 * k_ + 4].unsqueeze(2).to_broadcast([128, 4, 64]), ALU.mult)
            for c4 in range(4):
                ps = pn()
                kb.tr(ps.t[:, 0:128], ytm.t[:].rearrange("p h d -> p (h d)")[:, c4 * 128:(c4 + 1) * 128], ident_f)
                kb.cp('act', yout.t[:, c4, :], ps.t[:, 0:128])

        def ssd_stage(sc, l, Pm, TTc, NBK, NB, G, hT, yout, ST, STb, hist, last):
            L = TTc // NB
            zs = sc.sb("zs", [128, 4, TTc], F32)
            xpad = sc.sb("xpad", [128, 8, NB, L + 3], F32)
            xc = sc.sb("xc", [128, 8, TTc], BF16)
            dtm = sc.sb("dtm", [128, NBK, 8], F32)
            wz = G('z')
            proj_fm(hT, wz, TTc, 4, lambda j, p: kb.act(zs.t[:, j, :], p, AF.Silu))
            if Pm:
                kb.cp('pool', xpad.t[:, :, 0, 0:3], hist.t[:])
            else:
                c0 = sc.sb("c0", [48, 1024], F32)
                kb.dma('sp', c0.t[:], IN['conv0'][l])
                for ct in range(8):
                    ps = pn()
                    kb.tr(ps.t[:, 0:48], c0.t[:, ct * 128:(ct + 1) * 128], CA('ident', 48, 48))
                    kb.cp('act', xpad.t[:, ct, :, 0:3], ps.t[:, 0:48].rearrange("p (b j) -> p b j", j=3))
            for half in range(2):
                wx = G('xbc%d' % half)
                proj_fm(hT, wx, TTc, 4, lambda j, p, half=half: kb.cp('act', xpad.t[:, 4 * half + j, :, 3:3 + L], p.rearrange("p (b t) -> p b t", b=NB)))
            wd = G('dt')
            proj_tm(hT, wd, 8, NBK, lambda b, p: kb.tt('dve', dtm.t[:, b, :], p, PAR('dtb', 0, 8), ALU.add))
            softplus_inplace(dtm.t[:], sc, [128, NBK, 8])
            cacc = sc.sb("cacc", [128, NB, L], F32)
            ctmp = sc.sb("ctmp", [128, NB, L], F32)
            for ct in range(8):
                kb.ts('pool', cacc.t[:], xpad.t[:, ct, :, 0:L], PAR('convw', ct * 4), ALU.mult)
                for j in range(1, 4):
                    kb.ts('pool', ctmp.t[:], xpad.t[:, ct, :, j:j + L], PAR('convw', ct * 4 + j), ALU.mult)
                    kb.tt('pool', cacc.t[:], cacc.t[:], ctmp.t[:], ALU.add)
                kb.act(xc.t[:, ct, :].rearrange("p (b t) -> p b t", b=NB), cacc.t[:], AF.Silu, bias=PAR('convb', ct))
            if Pm:
                kb.cp('pool', hist.t[:], xpad.t[:, :, 0, L:L + 3])
            if last:
                nrow = 3 * NB
                cs = sc.sb("cs", [128, 8, nrow], F32)
                kb.cp('dve', cs.t[:].rearrange("p c (b j) -> p c b j", j=3), xpad.t[:, :, :, L:L + 3])
                co = sc.sb("co", [nrow, 1024], F32)
                for hf in range(2):
                    ps = pn()
                    for c4 in range(4):
                        kb.tr(ps.t[:nrow, c4 * 128:(c4 + 1) * 128], cs.t[:, hf * 4 + c4, :], ident_f)
                    kb.cp('act', co.t[:, hf * 512:(hf + 1) * 512], ps.t[:nrow, :])
                kb.dma('pool', OUT['conv_p' if Pm else 'conv_s'].t[l], co.t[:])
            tri = CA('tri_p' if Pm else 'tri_s')
            bd = CA('ones' if Pm else 'bd_s')
            sm = sc.sb("ssm", [128, 6, 8], F32)
            adtrep = sc.sb("adtrep", [128, 8, 128], F32)
            dd = sc.sb("dd", [128, 8, 128], F32)
            ea = sc.sb("ea", [128, 8, 128], F32)
            Gm = sc.sb("Gm", [128, 2, 128], F32)
            MT = sc.sb("MT", [128, 8, 128], BF16)
            Cea = sc.sb("Cea", [128, 8, 128], BF16)
            xd = sc.sb("xd", [128, 512], BF16)
            xdd = sc.sb("xdd", [128, 512], BF16)
            Btm = sc.sb("Btm", [128, 2, 128], BF16)
            y1 = sc.sb("y1", [128, 4, 128], F32)
            ysq = sc.sb("ysq", [128, 4, 128], F32)
            rstd = sc.sb("rstd", [128, 128], F32)
            H4 = lambda ap: ap.rearrange("p (g h) l -> p g h l", g=2)
            for c in range(NBK):
                tok = slice(c * 128, (c + 1) * 128)
                dt_c = dtm.t[:, c, :]
                adt, acum, atot, dte, dA = sm.t[:, 0, :], sm.t[:, 1, :], sm.t[:, 2, :], sm.t[:, 3, :], sm.t[:, 4, :]
                kb.tt('dve', adt, dt_c, PAR('arow', 0, 8), ALU.mult)
                psA = pn()
                kb.mm(psA.t[:, 0:8], tri, adt)
                kb.mm(psA.t[:, 8:16], bd, adt)
                kb.cp('dve', sm.t[:, 1:3, :], psA.t[:, 0:16].rearrange("p (k h) -> p k h", k=2))
                kb.tt('dve', dte, atot, acum, ALU.subtract)
                kb.act(dte, dte, AF.Exp)
                kb.cp('dve', adtrep.t[:], adt.unsqueeze(2).to_broadcast([128, 8, 128]))
                psB = [pn(), pn()]
                for h in range(8):
                    kb.mm(psB[h // 4].t[:, (h % 4) * 128:(h % 4 + 1) * 128], adtrep.t[:, h, :], tri)
                for k in range(2):
                    pv = psB[k].t[:, :].rearrange("p (h l) -> p h l", h=4)
                    kb.tt('dve', dd.t[:, 4 * k:4 * k + 4, :], pv, sm.t[:, 1, 4 * k:4 * k + 4].unsqueeze(2).to_broadcast([128, 4, 128]), ALU.subtract)
                    kb.act(ea.t[:, 4 * k:4 * k + 4, :], pv, AF.Exp)
                kb.ts('dve', dd.t[:], dd.t[:], 0.0, ALU.min)
                kb.act(dd.t[:], dd.t[:], AF.Exp)
                psG = pn()
                for g in range(2):
                    kb.mm(psG.t[:, g * 128:(g + 1) * 128], xc.t[:, 4 + g, tok], xc.t[:, 6 + g, tok])
                kb.tt('dve', Gm.t[:], psG.t[:, 0:256].rearrange("p (g l) -> p g l", g=2), tri.unsqueeze(1).to_broadcast([128, 2, 128]), ALU.mult)
                kb.tt('dve', H4(MT.t[:]), H4(dd.t[:]), Gm.t[:].unsqueeze(2).to_broadcast([128, 2, 4, 128]), ALU.mult)
                kb.tt('pool', H4(Cea.t[:]), H4(ea.t[:]), xc.t[:, 6:8, tok].unsqueeze(2).to_broadcast([128, 2, 4, 128]), ALU.mult)
                psX = pn()
                pbx = psX.t[:, :].bitcast(BF16)
                for ct in range(4):
                    kb.tr(pbx[:, ct * 128:(ct + 1) * 128], xc.t[:, ct, tok], ident_b)
                kb.tt('dve', xd.t[:].rearrange("p (h q) -> p h q", h=8), pbx[:, 0:512].rearrange("p (h q) -> p h q", h=8),
                      dt_c.unsqueeze(2).to_broadcast([128, 8, 64]), ALU.mult)
                kb.tt('dve', xdd.t[:].rearrange("p (h q) -> p h q", h=8), xd.t[:].rearrange("p (h q) -> p h q", h=8),
                      dte.unsqueeze(2).to_broadcast([128, 8, 64]), ALU.mult)
                psT = pn()
                pbt = psT.t[:, :].bitcast(BF16)
                for g in range(2):
                    kb.tr(pbt[:, g * 128:(g + 1) * 128], xc.t[:, 4 + g, tok], ident_b)
                kb.cp('act', Btm.t[:], pbt[:, 0:256].rearrange("p (g n) -> p g n", g=2))
                Y, Yoff = acc[0], acc[1]
                for h in range(8):
                    hp, po = h // 2, 64 * (h % 2)
                    kb.mm(Y.t[po:po + 64, hp * 128:(hp + 1) * 128], xd.t[:, h * 64:(h + 1) * 64], MT.t[:, h, :], start=True, stop=not Pm)
                    if Pm:
                        kb.mm(Y.t[po:po + 64, hp * 128:(hp + 1) * 128], STb.t[:, h * 64:(h + 1) * 64], Cea.t[:, h, :], start=False, stop=True)
                psS0 = None
                if Pm:
                    psD = pn()
                    kb.mm(psD.t[:, 0:8], CA('ones'), adt)
                    kb.act(dA, psD.t[:, 0:8], AF.Exp)
                    psS = pn()
                    for g in range(2):
                        kb.mm(psS.t[:, g * 256:(g + 1) * 256], Btm.t[:, g, :], xdd.t[:, g * 256:(g + 1) * 256])
                    kb.tt('dve', ST.t[:].rearrange("p (h q) -> p h q", h=8), ST.t[:].rearrange("p (h q) -> p h q", h=8),
                          dA.unsqueeze(2).to_broadcast([128, 8, 64]), ALU.mult)
                    kb.tt('dve', ST.t[:], ST.t[:], psS.t[:, :], ALU.add)
                    kb.cp('act', STb.t[:], ST.t[:])
                else:
                    radt = sc.sb("radt", [128, 16, 8], F32)
                    kb.tt('dve', radt.t[:], adt.unsqueeze(1).to_broadcast([128, 16, 8]), CA('inb', 16).unsqueeze(2).to_broadcast([128, 16, 8]), ALU.mult)
                    psD = pn()
                    kb.mm(psD.t[:, 0:128], CA('ones'), radt.t[:].rearrange("p b h -> p (b h)"))
                    dAb = sc.sb("dAb", [128, 16, 8], F32)
                    kb.act(dAb.t[:].rearrange("p b h -> p (b h)"), psD.t[:, 0:128], AF.Exp)
                    sin_ = [sc.sb("sin", [128, 4, 128], F32) for _ in range(2)]
                    stf = [sc.sb("stf", [128, 512], F32) for _ in range(2)]
                    stb_ = [sc.sb("stb", [128, 512], BF16) for _ in range(2)]
                    xdb = [sc.sb("xdb", [128, 512], BF16) for _ in range(2)]
                    sout = [sc.sb("sout", [128, 4, 128], F32) for _ in range(2)]
                    for b in range(16):
                        si_, sf_, sb_, xb_, so_ = sin_[b % 2], stf[b % 2], stb_[b % 2], xdb[b % 2], sout[b % 2]
                        kb.dma('sp', si_.t[:], IN['ssd0'][l, b].rearrange("(c q) n -> q c n", q=128))
                        ps = pn()
                        for c4 in range(4):
                            kb.tr(ps.t[:, c4 * 128:(c4 + 1) * 128], si_.t[:, c4, :], ident_f)
                        kb.cp('dve', sf_.t[:], ps.t[:, :])
                        kb.cp('act', sb_.t[:], ps.t[:, :])
                        for h in range(8):
                            hp, po = h // 2, 64 * (h % 2)
                            kb.mm(Yoff.t[po:po + 64, hp * 128 + b * 8:hp * 128 + b * 8 + 8], sb_.t[:, h * 64:(h + 1) * 64], Cea.t[:, h, b * 8:(b + 1) * 8])
                        kb.ts('dve', xb_.t[:], xdd.t[:], CA('inb', 1, 128, b), ALU.mult)
                        psS = pn()
                        for g in range(2):
                            kb.mm(psS.t[:, g * 256:(g + 1) * 256], Btm.t[:, g, :], xb_.t[:, g * 256:(g + 1) * 256])
                        kb.tt('dve', sf_.t[:].rearrange("p (h q) -> p h q", h=8), sf_.t[:].rearrange("p (h q) -> p h q", h=8),
                              dAb.t[:, b, :].unsqueeze(2).to_broadcast([128, 8, 64]), ALU.mult)
                        kb.tt('dve', sf_.t[:], sf_.t[:], psS.t[:, :], ALU.add)
                        ps2 = pn()
                        for c4 in range(4):
                            kb.tr(ps2.t[:, c4 * 128:(c4 + 1) * 128], sf_.t[:, c4 * 128:(c4 + 1) * 128], ident_f)
                        kb.cp('act', so_.t[:], ps2.t[:, :].rearrange("p (c n) -> p c n", c=4))
                        kb.dma('pool', OUT['ssd_s'].t[l, b].rearrange("(c q) n -> q c n", q=128), so_.t[:])
                kb.tt('dve', y1.t[:], xc.t[:, 0:4, tok], PAR('dcol', 0, 4).unsqueeze(2).to_broadcast([128, 4, 128]), ALU.mult)
                kb.tt('dve', y1.t[:], y1.t[:], Y.t[:, :].rearrange("p (h l) -> p h l", h=4), ALU.add)
                if not Pm:
                    kb.tt('dve', y1.t[:], y1.t[:], Yoff.t[:, :].rearrange("p (h l) -> p h l", h=4), ALU.add)
                kb.tt('dve', y1.t[:], y1.t[:], zs.t[:, :, tok], ALU.mult)
                kb.act(ysq.t[:], y1.t[:], AF.Square)
                psR = pn()
                for hp in range(4):
                    kb.mm(psR.t[:, 0:128], CA('ones'), ysq.t[:, hp, :], start=(hp == 0), stop=(hp == 3))
                kb.ts('dve', rstd.t[:], psR.t[:, 0:128], 1.0 / 512.0, ALU.mult, RMS_EPS, ALU.add)
                kb.act(rstd.t[:], rstd.t[:], AF.Sqrt)
                recip(rstd.t[:], rstd.t[:])
                kb.tt('dve', y1.t[:], y1.t[:], rstd.t[:].unsqueeze(1).to_broadcast([128, 4, 128]), ALU.mult)
                kb.tt('dve', yout.t[:, :, tok], y1.t[:], PAR('nw', 0, 4).unsqueeze(2).to_broadcast([128, 4, 128]), ALU.mult)
            if Pm and last:
                so_ = sc.sb("sso", [128, 4, 128], F32)
                ps2 = pn()
                for c4 in range(4):
                    kb.tr(ps2.t[:, c4 * 128:(c4 + 1) * 128], ST.t[:, c4 * 128:(c4 + 1) * 128], ident_f)
                kb.cp('act', so_.t[:], ps2.t[:, :].rearrange("p (c n) -> p c n", c=4))
                kb.dma('pool', OUT['ssd_p'].t[l].rearrange("(c q) n -> q c n", q=128), so_.t[:])

        try:
            for l in range(DEPTH):
                if cfg.skip_prompt:
                    for ti in range(NT):
                        for key, _, _, _ in [p_ for p_ in ws.plan if p_[0][0] == ('P', l, ti)]:
                            ws.get(key)
                    continue
                run_layer(l, 'P')
            ck(11)
            phase_base[0] = 20
            for l in range(DEPTH):
                run_layer(l, 'S')
            assert ws.pos == len(ws.plan), (ws.pos, len(ws.plan))
        except _Stop:
            while kb.open:
                kb.open[-1].__exit__(None, None, None)
        kb.drain()
    return nc


def core_inputs(inp, cfg, c, nprompt):
    NPOOL = cfg.NPOOL
    f32 = lambda a: np.ascontiguousarray(np.asarray(a, dtype=np.float32))
    m = {}
    m['xp'] = f32(inp['x_prompt'][c % nprompt])
    sl = slice(16 * c, 16 * (c + 1))
    m['xsm'] = f32(inp['x_sample'][sl]).reshape(128, D)
    for i in range(2):
        m['ck%d' % i] = np.asarray(inp['cache_k'], dtype=np.float32)[i].reshape(NPOOL * 128, 512)
        m['cv%d' % i] = np.asarray(inp['cache_v'], dtype=np.float32)[i].reshape(NPOOL * 128, 512)
        m['clf%d' % i] = np.asarray(inp['cache_logf'], dtype=np.float32)[i].reshape(NPOOL, 1024)
    m['ptab'] = np.ascontiguousarray(np.asarray(inp['page_table'], dtype=np.int32)[sl]).reshape(1, -1)
    m['s5re0'] = f32(inp['state_s5_re'][:, sl]).reshape(2, 16, 2048)
    m['s5im0'] = f32(inp['state_s5_im'][:, sl]).reshape(2, 16, 2048)
    m['conv0'] = f32(inp['state_conv'][:, sl]).reshape(2, 48, 1024)
    m['ssd0'] = f32(inp['state_ssd'][:, sl]).reshape(2, 16, 512, 128)
    return m


def shared_inputs(inp, cfg):
    m = {}
    for name, cnt, n in WEIGHT_SPECS:
        m[name] = np.asarray(inp[name], dtype=np.float32).reshape(cnt, n)
    for name, shp in SMALL_SPECS:
        m[name] = np.asarray(inp[name], dtype=np.float32).reshape(shp)
    cA, _, cB, _ = make_consts(cfg)
    m['cstA'] = cA
    m['cstB'] = cB
    return m


def assemble(res, ncore, nprompt):
    P = lambda k: [np.asarray(res[c][k]) for c in range(nprompt)]
    S = lambda k: [np.asarray(res[c][k]) for c in range(ncore)]
    seq = res[0]['y_p'].shape[0]
    y_prompt = np.stack(P('y_p'), 0)
    y_sample = np.concatenate([a.reshape(16, 8, D) for a in S('y_s')], 0)
    k_p = np.stack([a.reshape(2, seq, 8, 64) for a in P('k_p')], 1)
    v_p = np.stack([a.reshape(2, seq, 8, 64) for a in P('v_p')], 1)
    lf_p = np.stack(P('lf_p'), 1)
    s5re_p = np.stack([a.reshape(2, 32, 64) for a in P('s5re_p')], 1)
    s5im_p = np.stack([a.reshape(2, 32, 64) for a in P('s5im_p')], 1)
    conv_p = np.stack(P('conv_p'), 1)
    ssd_p = np.stack([a.reshape(2, 8, 64, 128) for a in P('ssd_p')], 1)
    k_s = np.concatenate([a.reshape(2, 16, 8, 8, 64) for a in S('k_s')], 1)
    v_s = np.concatenate([a.reshape(2, 16, 8, 8, 64) for a in S('v_s')], 1)
    lf_s = np.concatenate([a.reshape(2, 16, 8, 8) for a in S('lf_s')], 1)
    s5re_s = np.concatenate([a.reshape(2, 16, 32, 64) for a in S('s5re_s')], 1)
    s5im_s = np.concatenate([a.reshape(2, 16, 32, 64) for a in S('s5im_s')], 1)
    conv_s = np.concatenate([a.reshape(2, 16, 3, 1024) for a in S('conv_s')], 1)
    ssd_s = np.concatenate([a.reshape(2, 16, 8, 64, 128) for a in S('ssd_s')], 1)
    outs = (y_prompt, y_sample, k_p, v_p, lf_p, s5re_p, s5im_p, conv_p, ssd_p,
            k_s, v_s, lf_s, s5re_s, s5im_s, conv_s, ssd_s)
    return tuple(np.ascontiguousarray(o, dtype=np.float32) for o in outs)


def kernel(**inp):
    cfg = Cfg()
    nc = build(cfg)
    shared = shared_inputs(inp, cfg)
    in_maps = []
    for c in range(8):
        m = core_inputs(inp, cfg, c, 4)
        m.update(shared)
        in_maps.append(m)
    res = run_bass_kernel_spmd(nc, in_maps, core_ids=list(range(8)))
    return assemble(res.results, 8, 4)
```
